# Optimizing a Trainium2 kernel written in Bass

```python
import jax, jax.numpy as jnp
from jax import lax
import numpy as np

D_MODEL = 1024
BATCH = 16
SEQ = 4096
DEPTH = 2

CHUNK = 64
Q_BLOCK = 128
MEM_LEN = 256
MAX_STREAM_OFFSET = 4096
EPS = 1e-6
NEG_INF = -1e30

A_HEADS = 4
A_HEAD_DIM = 64
FORGET_BIAS_INIT = 3.0
B_GROUPS = 4
B_GROUP_DIM = 64
B_WINDOW = 128
C_HEADS = 8
C_NOPE_DIM = 64
C_ROPE_DIM = 32
C_V_DIM = 64
C_Q_RANK = 256
C_KV_RANK = 128
ROPE_THETA = 10000.0
M_HEADS = 4
M_HEAD_DIM = 128
D_FF = 2752
N_EXPERTS = 8
TOP_K = 2
D_FF_EXPERT = 1408

A_W = A_HEADS * A_HEAD_DIM
B_W = B_GROUPS * B_GROUP_DIM
C_W = C_HEADS * C_V_DIM
MIX_W = A_W + B_W + C_W
M_W = M_HEADS * M_HEAD_DIM
IN_SIZES = (A_W, A_W, A_W, A_HEADS, B_W, B_W, C_Q_RANK, C_KV_RANK, C_ROPE_DIM)
IN_W = int(sum(IN_SIZES))
IN_OFFSETS = tuple(int(v) for v in np.cumsum(IN_SIZES)[:-1])
N_DENSE = (DEPTH + 1) // 2
N_MOE = DEPTH // 2

kernel_name = 'hybrid_fox_gmlp_mla_moe_block'


def _rmsnorm(x, g):
    xf = x.astype(jnp.float32)
    y = xf * lax.rsqrt(jnp.mean(xf * xf, axis=-1, keepdims=True) + EPS)
    return (y * g.astype(jnp.float32)).astype(x.dtype)


def _layernorm(x, g):
    xf = x.astype(jnp.float32)
    mu = jnp.mean(xf, axis=-1, keepdims=True)
    var = jnp.mean(jnp.square(xf - mu), axis=-1, keepdims=True)
    return ((xf - mu) * lax.rsqrt(var + EPS) * g.astype(jnp.float32)).astype(x.dtype)


def _rope(x, pos):
    half = x.shape[-1] // 2
    inv = ROPE_THETA ** (-jnp.arange(half, dtype=jnp.float32) / half)
    ang = pos.astype(jnp.float32)[..., None] * inv
    ang = ang.reshape(ang.shape[:2] + (1,) * (x.ndim - 3) + (half,))
    cos, sin = jnp.cos(ang), jnp.sin(ang)
    x1 = x[..., :half].astype(jnp.float32)
    x2 = x[..., half:].astype(jnp.float32)
    return jnp.concatenate([x1 * cos - x2 * sin, x1 * sin + x2 * cos], axis=-1).astype(x.dtype)


def _swiglu(h, w_gate, w_up, w_down):
    return (jax.nn.silu(h @ w_gate) * (h @ w_up)) @ w_down


def _sweep_attention(q, k, v, scale, unit, log_decay):
    bn, nh, s_len, dk = q.shape
    nb = s_len // Q_BLOCK
    k_pos = jnp.arange(s_len)
    q_blocks = q.reshape(bn, nh, nb, Q_BLOCK, dk).transpose(2, 0, 1, 3, 4)
    if log_decay is None:
        xs = (q_blocks, jnp.arange(nb))
    else:
        xs = (q_blocks, jnp.arange(nb), log_decay.reshape(bn, nh, nb, Q_BLOCK).transpose(2, 0, 1, 3))

    def block(blk):
        qi, i = blk[0], blk[1]
        s = jnp.einsum('bhqd,bhkd->bhqk', qi, k, preferred_element_type=jnp.float32) * scale
        if log_decay is not None:
            s = s + blk[2][..., :, None] - log_decay[:, :, None, :]
        q_pos = i * Q_BLOCK + jnp.arange(Q_BLOCK)
        allowed = (k_pos[None, :] // unit) <= (q_pos[:, None] // unit)
        s = jnp.where(allowed, s, NEG_INF)
        p = jax.nn.softmax(s, axis=-1)
        return jnp.einsum('bhqk,bhkd->bhqd', p.astype(v.dtype), v)

    out = lax.map(block, xs)
    return out.transpose(1, 2, 0, 3, 4).reshape(bn, nh, s_len, v.shape[-1])


def _forgetting_attention(qa, ka, va, fa, b_forget, q_gain, k_gain):
    bn, s_len, _ = qa.shape
    q = _rmsnorm(qa.reshape(bn, s_len, A_HEADS, A_HEAD_DIM), q_gain)
    k = _rmsnorm(ka.reshape(bn, s_len, A_HEADS, A_HEAD_DIM), k_gain)
    v = va.reshape(bn, s_len, A_HEADS, A_HEAD_DIM)
    log_f = jax.nn.log_sigmoid((fa + b_forget).astype(jnp.float32))
    cum = jnp.cumsum(log_f, axis=1).transpose(0, 2, 1)
    o = _sweep_attention(q.transpose(0, 2, 1, 3), k.transpose(0, 2, 1, 3), v.transpose(0, 2, 1, 3),
                         A_HEAD_DIM ** -0.5, 1, cum)
    return o.transpose(0, 2, 1, 3).reshape(bn, s_len, A_W)


def _spatial_gating(ub, vb, v_gain, w_s, b_s):
    bn, s_len, _ = ub.shape
    u = jax.nn.gelu(ub, approximate=False)
    v = jax.nn.gelu(vb, approximate=False).reshape(bn, s_len, B_GROUPS, B_GROUP_DIM)
    v = _layernorm(v, v_gain.reshape(B_GROUPS, B_GROUP_DIM))
    v = v.reshape(bn, s_len // B_WINDOW, B_WINDOW, B_GROUPS, B_GROUP_DIM)
    pos = jnp.arange(B_WINDOW)
    mask = (pos[None, :] // CHUNK) <= (pos[:, None] // CHUNK)
    w = jnp.where(mask[None], w_s, 0.0)
    y = jnp.einsum('gts,bnsgd->bntgd', w, v) + b_s.T[None, None, :, :, None]
    return u * y.reshape(bn, s_len, B_W)


def _latent_attention(cq, ckv, kr, positions, cq_gain, w_uq, ckv_gain, w_ukv,
                      qn_g, qr_g, kn_g, kr_g):
    bn, s_len, _ = cq.shape
    q = jnp.einsum('bsr,rhd->bshd', _rmsnorm(cq, cq_gain), w_uq)
    q_nope = _rmsnorm(q[..., :C_NOPE_DIM], qn_g)
    q_rope = _rope(_rmsnorm(q[..., C_NOPE_DIM:], qr_g), positions)
    kv = jnp.einsum('bsr,rhd->bshd', _rmsnorm(ckv, ckv_gain), w_ukv)
    k_nope = _rmsnorm(kv[..., :C_NOPE_DIM], kn_g)
    v = kv[..., C_NOPE_DIM:]
    k_rope = _rope(_rmsnorm(kr, kr_g), positions)
    k = jnp.concatenate([k_nope, jnp.broadcast_to(k_rope[:, :, None, :],
                                                  (bn, s_len, C_HEADS, C_ROPE_DIM))], axis=-1)
    q = jnp.concatenate([q_nope, q_rope], axis=-1)
    o = _sweep_attention(q.transpose(0, 2, 1, 3), k.transpose(0, 2, 1, 3), v.transpose(0, 2, 1, 3),
                         (C_NOPE_DIM + C_ROPE_DIM) ** -0.5, CHUNK, None)
    return o.transpose(0, 2, 1, 3).reshape(bn, s_len, C_W)


def _memory_attention(h, mem_n, w_q, w_kv, q_g, k_g, w_o):
    bn, s_len, _ = h.shape
    m_len = mem_n.shape[1]
    q = _rmsnorm((h @ w_q).reshape(bn, s_len, M_HEADS, M_HEAD_DIM), q_g)
    kv = (mem_n @ w_kv).reshape(bn, m_len, 2, M_HEADS, M_HEAD_DIM)
    k = _rmsnorm(kv[:, :, 0], k_g)
    v = kv[:, :, 1]
    s = jnp.einsum('bshd,bmhd->bhsm', q, k, preferred_element_type=jnp.float32) * M_HEAD_DIM ** -0.5
    p = jax.nn.softmax(s, axis=-1)
    o = jnp.einsum('bhsm,bmhd->bshd', p.astype(v.dtype), v).reshape(bn, s_len, M_W)
    return o @ w_o


def _moe(h, w_router, b_router, w_gate, w_up, w_down):
    logits = (h @ w_router).astype(jnp.float32) + b_router.astype(jnp.float32)
    top_v, top_i = lax.top_k(logits, TOP_K)
    gates = jax.nn.softmax(top_v, axis=-1)
    combine = jnp.sum(jax.nn.one_hot(top_i, N_EXPERTS, dtype=jnp.float32) * gates[..., None],
                      axis=-2).astype(h.dtype)
    y = jnp.zeros_like(h)
    for e in range(N_EXPERTS):
        y = y + combine[..., e:e + 1] * _swiglu(h, w_gate[e], w_up[e], w_down[e])
    return y


def setup_inputs(seed: int = 0) -> dict:
    key = jax.random.key(seed)
    ks = iter(jax.random.split(key, 64))

    def nrm(shape, fan_in, mult=1.0):
        return mult * fan_in ** -0.5 * jax.random.normal(next(ks), shape, jnp.float32)

    def gain(shape):
        return 1.0 + 0.05 * jax.random.normal(next(ks), shape, jnp.float32)

    x = jax.random.normal(next(ks), (BATCH, SEQ, D_MODEL), jnp.float32)
    mem = jax.random.normal(next(ks), (BATCH, MEM_LEN, D_MODEL), jnp.float32)
    offsets = jax.random.randint(next(ks), (BATCH, 1), 0, MAX_STREAM_OFFSET, dtype=jnp.int32)
    positions = offsets + jnp.arange(SEQ, dtype=jnp.int32)[None, :]
    return {
        'x': x,
        'mem': mem,
        'positions': positions,
        'mix_norm': gain((DEPTH, D_MODEL)),
        'w_in': nrm((DEPTH, D_MODEL, IN_W), D_MODEL),
        'b_forget': FORGET_BIAS_INIT + 0.5 * jax.random.normal(next(ks), (DEPTH, A_HEADS), jnp.float32),
        'a_q_norm': gain((DEPTH, A_HEAD_DIM)),
        'a_k_norm': gain((DEPTH, A_HEAD_DIM)),
        'b_v_norm': gain((DEPTH, B_W)),
        'b_spatial_w': nrm((DEPTH, B_GROUPS, B_WINDOW, B_WINDOW), B_WINDOW, 0.5),
        'b_spatial_b': gain((DEPTH, B_GROUPS, B_WINDOW)),
        'c_q_lat_norm': gain((DEPTH, C_Q_RANK)),
        'c_w_uq': nrm((DEPTH, C_Q_RANK, C_HEADS, C_NOPE_DIM + C_ROPE_DIM), C_Q_RANK),
        'c_kv_lat_norm': gain((DEPTH, C_KV_RANK)),
        'c_w_ukv': nrm((DEPTH, C_KV_RANK, C_HEADS, C_NOPE_DIM + C_V_DIM), C_KV_RANK),
        'c_q_nope_norm': gain((DEPTH, C_NOPE_DIM)),
        'c_q_rope_norm': gain((DEPTH, C_ROPE_DIM)),
        'c_k_nope_norm': gain((DEPTH, C_NOPE_DIM)),
        'c_k_rope_norm': gain((DEPTH, C_ROPE_DIM)),
        'out_norm_a': gain((DEPTH, A_W)),
        'out_norm_b': gain((DEPTH, B_W)),
        'out_norm_c': gain((DEPTH, C_W)),
        'w_out': nrm((DEPTH, MIX_W, D_MODEL), MIX_W),
        'xattn_norm': gain((DEPTH, D_MODEL)),
        'mem_norm': gain((DEPTH, D_MODEL)),
        'w_mem_q': nrm((DEPTH, D_MODEL, M_W), D_MODEL),
        'w_mem_kv': nrm((DEPTH, D_MODEL, 2 * M_W), D_MODEL),
        'm_q_norm': gain((DEPTH, M_HEAD_DIM)),
        'm_k_norm': gain((DEPTH, M_HEAD_DIM)),
        'w_mem_out': nrm((DEPTH, M_W, D_MODEL), M_W),
        'ffn_norm': gain((DEPTH, D_MODEL)),
        'ffn_w_gate': nrm((N_DENSE, D_MODEL, D_FF), D_MODEL),
        'ffn_w_up': nrm((N_DENSE, D_MODEL, D_FF), D_MODEL),
        'ffn_w_down': nrm((N_DENSE, D_FF, D_MODEL), D_FF),
        'w_router': nrm((N_MOE, D_MODEL, N_EXPERTS), D_MODEL),
        'b_router': 0.01 * jax.random.normal(next(ks), (N_MOE, N_EXPERTS), jnp.float32),
        'moe_w_gate': nrm((N_MOE, N_EXPERTS, D_MODEL, D_FF_EXPERT), D_MODEL),
        'moe_w_up': nrm((N_MOE, N_EXPERTS, D_MODEL, D_FF_EXPERT), D_MODEL),
        'moe_w_down': nrm((N_MOE, N_EXPERTS, D_FF_EXPERT, D_MODEL), D_FF_EXPERT),
    }


def reference(x, mem, positions, mix_norm, w_in, b_forget, a_q_norm, a_k_norm, b_v_norm,
              b_spatial_w, b_spatial_b, c_q_lat_norm, c_w_uq, c_kv_lat_norm, c_w_ukv,
              c_q_nope_norm, c_q_rope_norm, c_k_nope_norm, c_k_rope_norm,
              out_norm_a, out_norm_b, out_norm_c, w_out, xattn_norm, mem_norm,
              w_mem_q, w_mem_kv, m_q_norm, m_k_norm, w_mem_out, ffn_norm,
              ffn_w_gate, ffn_w_up, ffn_w_down, w_router, b_router,
              moe_w_gate, moe_w_up, moe_w_down):
    for l in range(DEPTH):
        h = _rmsnorm(x, mix_norm[l])
        proj = h @ w_in[l]
        qa, ka, va, fa, ub, vb, cq, ckv, kr = jnp.split(proj, IN_OFFSETS, axis=-1)
        a = _forgetting_attention(qa, ka, va, fa, b_forget[l], a_q_norm[l], a_k_norm[l])
        b = _spatial_gating(ub, vb, b_v_norm[l], b_spatial_w[l], b_spatial_b[l])
        c = _latent_attention(cq, ckv, kr, positions, c_q_lat_norm[l], c_w_uq[l],
                              c_kv_lat_norm[l], c_w_ukv[l], c_q_nope_norm[l], c_q_rope_norm[l],
                              c_k_nope_norm[l], c_k_rope_norm[l])
        o = jnp.concatenate([_rmsnorm(a, out_norm_a[l]), _rmsnorm(b, out_norm_b[l]),
                             _rmsnorm(c, out_norm_c[l])], axis=-1)
        x = x + o @ w_out[l]
        x = x + _memory_attention(_rmsnorm(x, xattn_norm[l]), _rmsnorm(mem, mem_norm[l]),
                                  w_mem_q[l], w_mem_kv[l], m_q_norm[l], m_k_norm[l], w_mem_out[l])
        h = _rmsnorm(x, ffn_norm[l])
        if l % 2 == 0:
            x = x + _swiglu(h, ffn_w_gate[l // 2], ffn_w_up[l // 2], ffn_w_down[l // 2])
        else:
            m = l // 2
            x = x + _moe(h, w_router[m], b_router[m], moe_w_gate[m], moe_w_up[m], moe_w_down[m])
    return x
```

```python
import contextlib
import numpy as np
import concourse.bass as bass
import concourse.mybir as mybir
from concourse.bass_utils import run_bass_kernel_spmd

F32 = mybir.dt.float32
BF16 = mybir.dt.bfloat16
I32 = mybir.dt.int32
ALU = mybir.AluOpType
AF = mybir.ActivationFunctionType
AX = mybir.AxisListType

COMPUTE = ("pe", "act", "dve", "pool")
DMAQ = ("sp", "dq")
STREAMS = COMPUTE + DMAQ
EPOCH = 12000
NDSEM = {"sp": 24, "dq": 12}


class Op:
    __slots__ = ("eng", "fn", "rk", "wk", "deps", "idx", "seq", "sig", "is_dma", "waits", "dslot")

    def __init__(self, eng, fn, rk, wk, is_dma):
        self.eng = eng
        self.fn = fn
        self.rk = rk
        self.wk = wk
        self.is_dma = is_dma
        self.deps = set()
        self.sig = None
        self.waits = []


def _key(ap):
    return ap.tensor.name


def _is_dram(ap):
    return type(ap.tensor).__name__.startswith("DRam")


class Prog:
    def __init__(self, nc):
        self.nc = nc
        self.ops = []
        self.pending = {s: set() for s in STREAMS}
        self.last_on = {}
        self.dma_since_bar = []
        self.phase_stack = None
        self.nphase = 0
        self.bar_aps = None
        self.split = {}

    def split1(self, t, stride):
        self.split[t.name] = stride
        return t

    def _keys(self, ap):
        name = ap.tensor.name
        st = self.split.get(name)
        if st is None:
            return [name]
        dims = ap.ap
        foff = ap.offset % dims[0][0]
        ext = 1
        for step, cnt in dims[1:]:
            ext += (cnt - 1) * step
        return [(name, k) for k in range(foff // st, (foff + ext - 1) // st + 1)]

    @contextlib.contextmanager
    def phase(self):
        st = contextlib.ExitStack()
        self.phase_stack = st
        self.nphase += 1
        try:
            yield
        finally:
            self.barrier()
            st.close()
            self.phase_stack = None

    def sb(self, name, shape, dt):
        return self.phase_stack.enter_context(self.nc.sbuf_tensor(f"p{self.nphase}_{name}", list(shape), dt))

    def ps(self, name, shape=(128, 512), dt=F32):
        return self.phase_stack.enter_context(self.nc.psum_tensor(f"p{self.nphase}_{name}", list(shape), dt))

    def barrier(self):
        d = set(self.last_on.values())
        for q in DMAQ:
            lst = [i for i in self.dma_since_bar if self.ops[i].eng == q]
            d |= set(lst[-NDSEM[q]:])
        bar_dst, bar_src = self.bar_aps
        m = self._add("sp", lambda e: e.dma_start(out=bar_dst, in_=bar_src), [], [], rk=[], wk=[], is_dma=True)
        m.deps |= d
        for s in STREAMS:
            self.pending[s] = {m.idx}
        self.dma_since_bar = [m.idx]

    def _add(self, eng, fn, reads, writes, rk=None, wk=None, is_dma=False):
        r = list(rk) if rk is not None else []
        w = list(wk) if wk is not None else []
        if rk is None:
            for a in reads:
                if a is not None and hasattr(a, "tensor"):
                    r += self._keys(a)
        if wk is None:
            for a in writes:
                if a is not None and hasattr(a, "tensor"):
                    w += self._keys(a)
        op = Op(eng, fn, r, w, is_dma)
        op.idx = len(self.ops)
        if self.pending[eng]:
            op.deps |= self.pending[eng]
            self.pending[eng] = set()
        self.ops.append(op)
        if is_dma:
            self.dma_since_bar.append(op.idx)
        else:
            self.last_on[eng] = op.idx
        return op

    def mm(self, out, lhsT, rhs, start=True, stop=True, **kw):
        return self._add("pe", lambda e: e.matmul(out, lhsT, rhs, start=start, stop=stop),
                         [lhsT, rhs], [out], **kw)

    def act(self, out, in_, func, bias=None, scale=None, **kw):
        def fn(e):
            kws = {}
            if bias is not None:
                kws["bias"] = bias
            if scale is not None:
                kws["scale"] = scale
            return e.activation(out, in_, func, **kws)
        return self._add("act", fn, [in_, bias, scale], [out], **kw)

    def tt(self, eng, out, in0, in1, op, **kw):
        return self._add(eng, lambda e: e.tensor_tensor(out, in0, in1, op), [in0, in1], [out], **kw)

    def ts(self, eng, out, in0, s1, op0, s2=None, op1=None, **kw):
        def fn(e):
            if op1 is None:
                return e.tensor_scalar(out, in0, s1, None, op0)
            return e.tensor_scalar(out, in0, s1, s2, op0, op1)
        return self._add(eng, fn, [in0, s1, s2], [out], **kw)

    def stt(self, out, in0, scalar, in1, op0, op1, **kw):
        return self._add("dve", lambda e: e.scalar_tensor_tensor(out, in0, scalar, in1, op0, op1),
                         [in0, scalar, in1], [out], **kw)

    def copy(self, eng, out, in_, **kw):
        if eng == "act":
            return self._add("act", lambda e: e.copy(out, in_), [in_], [out], **kw)
        return self._add(eng, lambda e: e.tensor_copy(out, in_), [in_], [out], **kw)

    def memset(self, eng, ap, val, **kw):
        return self._add(eng, lambda e: e.memset(ap, val), [], [ap], **kw)

    def recip(self, out, in_, **kw):
        return self._add("dve", lambda e: e.reciprocal(out, in_), [in_], [out], **kw)

    def reduce(self, eng, out, in_, op, axis=AX.X, **kw):
        return self._add(eng, lambda e: e.tensor_reduce(out, in_, axis, op), [in_], [out], **kw)

    def scan(self, out, d0, d1, initial, op0, op1, **kw):
        return self._add("dve", lambda e: e.tensor_tensor_scan(out, d0, d1, initial, op0, op1),
                         [d0, d1, initial], [out], **kw)

    def dma(self, q, out, in_, rk=None, wk=None):
        r = [] if _is_dram(in_) else self._keys(in_)
        w = [] if _is_dram(out) else self._keys(out)
        return self._add(q, lambda e: e.dma_start(out=out, in_=in_), [], [], rk=r, wk=w, is_dma=True)

    def _analyze(self):
        ops = self.ops
        last_w = {}
        readers = {}
        for op in ops:
            deps = op.deps
            for k in op.rk:
                if k in last_w:
                    deps.add(last_w[k])
            for k in op.wk:
                if k in last_w:
                    deps.add(last_w[k])
                for r in readers.get(k, ()):
                    deps.add(r)
            deps.discard(op.idx)
            for k in op.rk:
                readers.setdefault(k, []).append(op.idx)
            for k in op.wk:
                last_w[k] = op.idx
                readers[k] = []
        seqc = {}
        for op in ops:
            op.seq = seqc.get(op.eng, 0)
            seqc[op.eng] = op.seq + 1
        dcount = {q: 0 for q in DMAQ}
        dhist = {q: [] for q in DMAQ}
        for op in ops:
            if op.is_dma:
                q = op.eng
                n = NDSEM[q]
                i = dcount[q]
                op.dslot = i % n
                if i >= n:
                    op.deps.add(dhist[q][i - n])
                dhist[q].append(op.idx)
                dcount[q] = i + 1
        clock = {e: {} for e in STREAMS}
        known_dma = {e: set() for e in STREAMS}
        opclock = [None] * len(ops)
        needed = set()
        for op in ops:
            me = op.eng
            ck = clock[me]
            kd = known_dma[me]
            waits = []
            rks = None
            for d in sorted(op.deps):
                dop = ops[d]
                if dop.is_dma:
                    if d in kd:
                        continue
                    waits.append(d)
                    kd.add(d)
                    for s, v in opclock[d].items():
                        if ck.get(s, -1) < v:
                            ck[s] = v
                else:
                    if dop.eng == me:
                        if me == "pe":
                            continue
                        if rks is None:
                            rks = set(op.rk)
                        if not (rks & set(dop.wk)):
                            continue
                        if ck.get(me, -1) >= dop.seq:
                            continue
                        waits.append(d)
                        ck[me] = dop.seq
                        continue
                    if ck.get(dop.eng, -1) >= dop.seq:
                        continue
                    waits.append(d)
                    for s, v in opclock[d].items():
                        if ck.get(s, -1) < v:
                            ck[s] = v
                    ck[dop.eng] = max(ck.get(dop.eng, -1), dop.seq)
            op.waits = waits
            needed.update(waits)
            opclock[op.idx] = dict(ck)
        return needed

    def emit(self, final_wait_ops=()):
        nc = self.nc
        needed = self._analyze()
        for f in final_wait_ops:
            needed.add(f.idx)
        cnt = {e: 0 for e in COMPUTE}
        dcnt = {q: [0] * NDSEM[q] for q in DMAQ}
        for op in self.ops:
            if op.is_dma:
                dcnt[op.eng][op.dslot] += 16
                op.sig = ("d", op.eng, op.dslot, dcnt[op.eng][op.dslot])
            elif op.idx in needed:
                c = cnt[op.eng]
                op.sig = ("c", op.eng, c // EPOCH, c % EPOCH + 1)
                cnt[op.eng] = c + 1
        with contextlib.ExitStack() as st:
            sems = {}
            for e in COMPUTE:
                for ep in range(cnt[e] // EPOCH + 1):
                    sems[("c", e, ep)] = st.enter_context(nc.semaphore(f"s_{e}_{ep}"))
            for q in DMAQ:
                for s in range(NDSEM[q]):
                    sems[("d", q, s)] = st.enter_context(nc.semaphore(f"d_{q}_{s}"))
            block = st.enter_context(nc.Block())
            per = {e: [op for op in self.ops if op.eng == e] for e in STREAMS}
            ops = self.ops

            def run(e, lst, extra_final=()):
                for op in lst:
                    for d in op.waits:
                        sg = ops[d].sig
                        e.wait_ge(sems[sg[:3]], sg[3])
                    ins = op.fn(e)
                    if op.sig is not None:
                        ins.then_inc(sems[op.sig[:3]], 16 if op.is_dma else 1)
                for f in extra_final:
                    sg = f.sig
                    e.wait_ge(sems[sg[:3]], sg[3])

            @block.tensor
            def _(e):
                run(e, per["pe"])

            @block.scalar
            def _(e):
                run(e, per["act"])

            @block.vector
            def _(e):
                run(e, per["dve"])

            @block.gpsimd
            def _(e):
                run(e, sorted(per["pool"] + per["dq"], key=lambda o: o.idx))

            @block.sync
            def _(e):
                run(e, per["sp"], extra_final=final_wait_ops)
        return cnt


D = 1024
SEQ = 4096
NBC = 2
T = NBC * SEQ
CH = 512
NCH = T // CH
CPB = SEQ // CH
DEPTH = 2
EPS = 1e-6
IN_W = 1700
WIN_N = 1808
OFF_AQ, OFF_AK, OFF_AV, OFF_AF, OFF_BU, OFF_BV, OFF_CQ, OFF_CKV, OFF_KR = 0, 256, 512, 768, 772, 1028, 1284, 1540, 1668
OFF_F3, OFF_KRP = 1700, 1712
D_FF = 2752
D_FFE = 1408
NE = 8
MEM = 256

C_ID = 0
C_BD96 = 128
C_PERM = 224
C_SELQ = 320
C_SELK = 600
C_M3 = 880
C_INV = 883
C_SGN = 884
C_MASKA = 885
C_MASKC = 1013
C_SEL65 = 1141
C_SELE = 1205
CW = 1205 + 8 * 128


def build_consts():
    c = np.zeros((128, CW), np.float32)
    c[:, C_ID:C_ID + 128] = np.eye(128, dtype=np.float32)
    c[0:64, C_BD96:C_BD96 + 64] = 1.0 / 64
    c[64:96, C_BD96 + 64:C_BD96 + 96] = 1.0 / 32
    for m in range(96):
        src = m if m < 64 else (m + 16 if m < 80 else m - 16)
        c[src, C_PERM + m] = 1.0
    for h in range(4):
        q0 = C_SELQ + h * 70
        c[12, q0 + 64:q0 + 67] = 1.0
        c[h, q0 + 67] = 1.0
        c[4 + h, q0 + 68] = 1.0
        c[8 + h, q0 + 69] = 1.0
        k0 = C_SELK + h * 70
        c[h, k0 + 64] = -1.0
        c[4 + h, k0 + 65] = -1.0
        c[8 + h, k0 + 66] = -1.0
        c[12, k0 + 67:k0 + 70] = 1.0
    c[0:4, C_M3 + 0] = 1.0
    c[4:8, C_M3 + 1] = 1.0
    c[8:12, C_M3 + 2] = 1.0
    half = 16
    inv = (np.float32(10000.0) ** (-np.arange(half, dtype=np.float32) / np.float32(half))).astype(np.float32)
    c[64:80, C_INV] = inv
    c[80:96, C_INV] = inv
    c[64:80, C_SGN] = -1.0
    c[80:96, C_SGN] = 1.0
    p = np.arange(128)[:, None]
    t = np.arange(128)[None, :]
    c[:, C_MASKA:C_MASKA + 128] = (p <= t).astype(np.float32)
    c[:, C_MASKC:C_MASKC + 128] = ((p // 64) <= (t // 64)).astype(np.float32)
    c[64, C_SEL65:C_SEL65 + 64] = 1.0
    for e in range(8):
        c[e, C_SELE + e * 128:C_SELE + (e + 1) * 128] = 1.0
    return c


PARAM_SPECS = [
    ("mix_norm", (2, 1024)), ("w_in", (2, 1024, 1700)), ("b_forget", (2, 4)), ("a_q_norm", (2, 64)),
    ("a_k_norm", (2, 64)), ("b_v_norm", (2, 256)), ("b_spatial_w", (2, 4, 128, 128)), ("b_spatial_b", (2, 4, 128)),
    ("c_q_lat_norm", (2, 256)), ("c_w_uq", (2, 256, 768)), ("c_kv_lat_norm", (2, 128)), ("c_w_ukv", (2, 128, 1024)),
    ("c_q_nope_norm", (2, 64)), ("c_q_rope_norm", (2, 32)), ("c_k_nope_norm", (2, 64)), ("c_k_rope_norm", (2, 32)),
    ("out_norm_a", (2, 256)), ("out_norm_b", (2, 256)), ("out_norm_c", (2, 512)), ("w_out", (2, 1024, 1024)),
    ("xattn_norm", (2, 1024)), ("mem_norm", (2, 1024)), ("w_mem_q", (2, 1024, 512)), ("w_mem_kv", (2, 1024, 1024)),
    ("m_q_norm", (2, 128)), ("m_k_norm", (2, 128)), ("w_mem_out", (2, 512, 1024)), ("ffn_norm", (2, 1024)),
    ("ffn_w_gate", (1, 1024, 2752)), ("ffn_w_up", (1, 1024, 2752)), ("ffn_w_down", (1, 2752, 1024)),
    ("w_router", (1, 1024, 8)), ("b_router", (1, 8)), ("moe_w_gate", (1, 8, 1024, 1408)),
    ("moe_w_up", (1, 8, 1024, 1408)), ("moe_w_down", (1, 8, 1408, 1024)),
]


def col1(ap1d):
    return ap1d.rearrange("(p o) -> p o", o=1)


class Builder:
    def __init__(self, dbg=None, layers=(0, 1), stop_after=None):
        self.dbg = dbg or ()
        self.stop_after = stop_after
        self.layers = layers
        nc = bass.Bass("TRN2", target_bir_lowering=False)
        self.nc = nc
        self.P = Prog(nc)
        self.I = {}
        self.I["x"] = nc.dram_tensor("x", [T, D], F32, kind="ExternalInput").ap()
        self.I["mem"] = nc.dram_tensor("mem", [NBC * MEM, D], F32, kind="ExternalInput").ap()
        self.I["positions"] = nc.dram_tensor("positions", [NBC, SEQ], I32, kind="ExternalInput").ap()
        self.I["cst"] = nc.dram_tensor("cst", [128, CW], F32, kind="ExternalInput").ap()
        for name, shp in PARAM_SPECS:
            self.I[name] = nc.dram_tensor(name, list(shp), F32, kind="ExternalInput").ap()
        self.out = nc.dram_tensor("out", [T, D], F32, kind="ExternalOutput").ap()
        bar = nc.dram_tensor("bar_scratch", [1, 16], F32, kind="Internal").ap()
        self.P.bar_aps = (bar, self.I["cst"][0:1, 0:16])
        self.S = {}
        self.final_ops = []

    def scratch(self, name, shape, dt):
        kind = "ExternalOutput" if name in self.dbg else "Internal"
        ap = self.nc.dram_tensor(name, list(shape), dt, kind=kind).ap()
        self.S[name] = ap
        return ap

    def build(self):
        P = self.P
        S = self.scratch
        S("xT", [D, T], F32)
        S("hT3", [D, T], BF16)
        S("oT", [D, T], BF16)
        S("qA", [NBC, 70, 4, SEQ], BF16)
        S("kA", [NBC, 70, 4, SEQ], BF16)
        S("vA", [NBC, 128, 4, 32 * 65], BF16)
        S("qC", [NBC, 96, 8, SEQ], BF16)
        S("kC", [NBC, 96, 8, SEQ], BF16)
        S("vC", [NBC, 128, 8, 32 * 65], BF16)
        S("combT", [NE, T], F32)
        for l in range(DEPTH):
            S(f"Win{l}", [8, 128, WIN_N], BF16)
            S(f"Wuq{l}", [2, 128, 768], BF16)
            S(f"Wukv{l}", [1, 128, 1024], BF16)
            S(f"Wout{l}", [8, 128, 1024], BF16)
            S(f"Wmq{l}", [8, 128, 512], BF16)
            S(f"Wmkv{l}", [8, 128, 1024], BF16)
            S(f"Wmo{l}", [4, 128, 1024], BF16)

        stages = []
        stages.append(("P0", self.phase_cast))
        for l in self.layers:
            stages.append((f"P1_{l}", lambda l=l: self.phase_proj(l)))
            stages.append((f"P2_{l}", lambda l=l: self.phase_attn(l)))
            stages.append((f"P3_{l}", lambda l=l: self.phase_mix_out(l)))
            stages.append((f"P4_{l}", lambda l=l: self.phase_ffn_all(l)))
        stages.append(("PF", self.phase_final))
        for name, fn in stages:
            with P.phase():
                fn()
            if self.stop_after == name:
                break
        if not self.final_ops:
            pass
        cnt = P.emit(final_wait_ops=self.final_ops)
        return cnt

    def phase_cast(self):
        P, I, S = self.P, self.I, self.S
        NMAX = WIN_N
        si = [P.sb(f"si{i}", [128, NMAX], F32) for i in range(3)]
        so = [P.sb(f"so{i}", [128, NMAX], BF16) for i in range(3)]
        gcol = P.sb("gcol", [128, 128], F32)
        self._cast_i = 0
        self._gc = 0

        def load_gain(g1d, n):
            cols = []
            for kc in range((n + 127) // 128):
                rows = min(128, n - kc * 128)
                j = self._gc
                self._gc += 1
                P.dma("sp", gcol[0:rows, j:j + 1], col1(g1d[kc * 128:kc * 128 + rows]))
                cols.append(gcol[0:rows, j:j + 1])
            return cols

        def cast(src2d, dst, K, N, gains=None, special=None, Nd=None):
            Nd = Nd or N
            for kc in range((K + 127) // 128):
                rows = min(128, K - kc * 128)
                i = self._cast_i
                self._cast_i += 1
                a, o = si[i % 3], so[i % 3]
                eng = ("dve", "act")[i % 2]
                P.dma("sp", a[0:rows, 0:N], src2d[kc * 128:kc * 128 + rows, :])
                g = gains[kc] if gains is not None else None
                if special is not None:
                    special(a, o, g, rows, eng)
                elif g is None:
                    P.copy(eng, o[0:rows, 0:N], a[0:rows, 0:N])
                elif eng == "act":
                    P.act(o[0:rows, 0:N], a[0:rows, 0:N], AF.Copy, scale=g)
                else:
                    P.ts(eng, o[0:rows, 0:N], a[0:rows, 0:N], g, ALU.mult)
                P.dma("dq", dst[kc, 0:rows, :], o[0:rows, 0:Nd])

        def scaled(eng, out, in_, g):
            if eng == "act":
                P.act(out, in_, AF.Copy, scale=g)
            else:
                P.ts(eng, out, in_, g, ALU.mult)

        for l in range(DEPTH):
            if l not in self.layers:
                continue
            g = load_gain(I["mix_norm"][l], 1024)

            def sp_win(a, o, g, rows, eng):
                scaled(eng, o[:, 0:IN_W], a[:, 0:IN_W], g)
                for r in range(3):
                    P.ts("dve", o[:, OFF_F3 + 4 * r:OFF_F3 + 4 * r + 4], a[:, OFF_AF:OFF_AF + 4], g, ALU.mult)
                P.memset("pool", o[:, OFF_KRP:OFF_KRP + 64], 0.0)
                P.ts("dve", o[:, OFF_KRP + 64:OFF_KRP + 96], a[:, OFF_KR:OFF_KR + 32], g, ALU.mult)
            cast(I["w_in"][l], S[f"Win{l}"], 1024, IN_W, g, special=sp_win, Nd=WIN_N)
            g = load_gain(I["c_q_lat_norm"][l], 256)
            cast(I["c_w_uq"][l], S[f"Wuq{l}"], 256, 768, g)
            g = load_gain(I["c_kv_lat_norm"][l], 128)

            def sp_ukv(a, o, g, rows, eng):
                av = a[:, 0:1024].rearrange("p (h t d) -> p h t d", h=8, t=2)
                for t in range(2):
                    ov = o[:, t * 512:(t + 1) * 512].rearrange("p (h d) -> p h d", h=8)
                    P.ts("dve", ov, av[:, :, t, :], g, ALU.mult)
            cast(I["c_w_ukv"][l], S[f"Wukv{l}"], 128, 1024, g, special=sp_ukv)
            g = load_gain(I["out_norm_a"][l], 256) + load_gain(I["out_norm_b"][l], 256) + load_gain(I["out_norm_c"][l], 512)
            cast(I["w_out"][l], S[f"Wout{l}"], 1024, 1024, g)
            g = load_gain(I["xattn_norm"][l], 1024)
            cast(I["w_mem_q"][l], S[f"Wmq{l}"], 1024, 512, g)
            g = load_gain(I["mem_norm"][l], 1024)
            cast(I["w_mem_kv"][l], S[f"Wmkv{l}"], 1024, 1024, g)
            cast(I["w_mem_out"][l], S[f"Wmo{l}"], 512, 1024, None)

    def load_consts(self):
        P = self.P
        cf = P.sb("cstf", [128, CW], F32)
        cb = P.sb("cstb", [128, CW], BF16)
        P.dma("sp", cf[:], self.I["cst"])
        P.copy("dve", cb[:], cf[:])
        return cf, cb

    def const_tile(self, name, shape, val, dt=BF16):
        t = self.P.sb(name, shape, dt)
        self.P.memset("pool", t[:], val)
        return t

    def rstd_from_ms(self, ms_psum, M, out_rstd, tmp):
        P = self.P
        P.act(tmp, ms_psum, AF.Ln, bias=self.eps_col[0:M, 0:1])
        P.act(out_rstd, tmp, AF.Exp, scale=-0.5)

    def phase_proj(self, l):
        P, I, S = self.P, self.I, self.S
        cf, cb = self.load_consts()
        ident_f = cf[:, C_ID:C_ID + 128]
        self.eps_col = self.const_tile("epsc", [128, 1], EPS, F32)
        halfpi = self.const_tile("halfpi", [128, 1], float(np.pi / 2), F32)
        ones1024 = self.const_tile("on1024", [128, 128], 1.0 / 1024)
        ones256 = self.const_tile("on256", [128, 128], 1.0 / 256)
        ones128 = self.const_tile("on128", [128, 128], 1.0 / 128)
        ones64 = self.const_tile("on64", [64, 64], 1.0 / 64)
        bd96 = cb[0:96, C_BD96:C_BD96 + 96]
        perm96 = cb[0:96, C_PERM:C_PERM + 96]
        Win = P.sb("Win", [128, 8, WIN_N], BF16)
        P.dma("sp", Win[:], S[f"Win{l}"].rearrange("k p n -> p k n"))
        Wuq = P.sb("Wuq", [128, 2, 768], BF16)
        P.dma("sp", Wuq[:], S[f"Wuq{l}"].rearrange("k p n -> p k n"))
        Wukv = P.sb("Wukv", [128, 1024], BF16)
        P.dma("sp", Wukv[:], S[f"Wukv{l}"][0])
        prm = P.sb("prm", [128, 16], F32)
        P.memset("pool", prm[:], 0.0)
        P.dma("sp", prm[0:64, 0:1], col1(I["a_q_norm"][l]))
        P.dma("sp", prm[0:64, 1:2], col1(I["a_k_norm"][l]))
        P.dma("sp", prm[0:64, 2:3], col1(I["c_q_nope_norm"][l]))
        P.dma("sp", prm[64:96, 2:3], col1(I["c_q_rope_norm"][l]))
        P.dma("sp", prm[0:64, 3:4], col1(I["c_k_nope_norm"][l]))
        P.dma("sp", prm[64:96, 4:5], col1(I["c_k_rope_norm"][l]))
        for r in range(3):
            P.dma("sp", prm[4 * r:4 * r + 4, 5:6], col1(I["b_forget"][l]))
        gq_a = P.sb("gq_a", [64, 1], F32)
        P.ts("dve", gq_a[:], prm[0:64, 0:1], 64.0 ** -0.5, ALU.mult)
        gk_a = prm[0:64, 1:2]
        g96q = P.sb("g96q", [96, 1], F32)
        P.ts("dve", g96q[:], prm[0:96, 2:3], 96.0 ** -0.5, ALU.mult)
        gkn = prm[0:64, 3:4]
        g96kr = prm[0:96, 4:5]
        nbf = P.sb("nbf", [12, 1], F32)
        P.ts("dve", nbf[:], prm[0:12, 5:6], -1.0, ALU.mult)
        bvg = P.sb("bvg", [128, 256], F32)
        P.dma("sp", bvg[:], I["b_v_norm"][l:l + 1, :].partition_broadcast(128))
        bsb = P.sb("bsb", [64, 4, 128], F32)
        for g_ in range(4):
            P.dma("sp", bsb[:, g_, :], I["b_spatial_b"][l, g_:g_ + 1, :].partition_broadcast(64))
        wmT = P.sb("wmT", [128, 4, 128], BF16)
        wsf = P.sb("wsf", [128, 4, 128], F32)
        P.dma("sp", wsf[:], I["b_spatial_w"][l].rearrange("g t s -> t g s"))
        NSLOT = 5
        pa = [P.ps(f"pa{i}") for i in range(NSLOT)]
        pm = pa
        px = P.ps("px")
        pv = P.ps("pv", (128, 1024))
        pst = pa[0]
        for g_ in range(4):
            P.mm(pst[:, g_ * 128:(g_ + 1) * 128], wsf[:, g_, :], ident_f)
        maskc = cf[:, C_MASKC:C_MASKC + 128]
        for g_ in range(4):
            P.tt("dve", wmT[:, g_, :], pst[:, g_ * 128:(g_ + 1) * 128], maskc, ALU.mult)
        xtm = [P.sb(f"xtm{i}", [128, D], F32) for i in range(2)] if l == 0 else None
        xc = [P.sb(f"xc{i}", [128, 8, CH], F32) for i in range(1 if l == 0 else 2)]
        sqx = [P.sb(f"sqx{i}", [128, CH], BF16) for i in range(2)]
        hT = P.split1(P.sb("hT", [128, 8, CH], BF16), CH)
        qf = [P.sb(f"qf{i}", [128, CH], F32) for i in range(NSLOT)]
        sqt = [P.sb(f"sqt{i}", [128, CH], BF16) for i in range(NSLOT)]
        rs_t = [P.sb(f"rs_t{i}", [128, CH], F32) for i in range(NSLOT)]
        qn_t = [P.sb(f"qn_t{i}", [96, CH], BF16) for i in range(NSLOT)]
        rstd_x = rs_t[0]

        qaugA = P.split1(P.sb("qaugA", [70, 4, CH], BF16), CH)
        kaugA = P.split1(P.sb("kaugA", [70, 4, CH], BF16), CH)
        vAt = P.sb("vAt", [128, 4, 4, 65], BF16)
        vCt = P.sb("vCt", [128, 8, 4, 65], BF16)
        P.memset("pool", vAt[:], 1.0)
        P.memset("pool", vCt[:], 1.0)
        qCt = P.split1(P.sb("qCt", [96, 8, CH], BF16), CH)
        kCt = P.split1(P.sb("kCt", [96, 8, CH], BF16), CH)
        oBt = P.split1(P.sb("oBt", [64, 4, CH], BF16), CH)
        cqn = P.sb("cqn", [128, 2, CH], BF16)
        ckvn = P.sb("ckvn", [128, CH], BF16)
        krf = P.sb("krf", [96, CH], BF16)
        Fs = [P.sb(f"Fs{i}", [12, CH], F32) for i in range(2)]
        ef = P.sb("ef", [12, CH], F32)
        onesf = self.const_tile("onesf", [12, CH], 1.0, F32)
        hi = P.sb("hi", [12, CH], BF16)
        lo = P.sb("lo", [12, CH], BF16)
        lo2 = P.sb("lo2", [12, CH], BF16)
        r1 = P.sb("r1", [12, CH], F32)
        c1 = P.sb("c1", [12, CH], F32)
        Faug = self.const_tile("Faug", [13, CH], 1.0, BF16)
        m3 = cf[0:12, C_M3:C_M3 + 3]
        posi = P.sb("posi", [96, CH], I32)
        ang = P.sb("ang", [96, CH], F32)
        ang2 = P.sb("ang2", [96, CH], F32)
        uu = P.sb("uu", [96, CH], F32)
        Ctab = P.sb("Ctab", [96, CH], F32)
        Stab = P.sb("Stab", [96, CH], F32)
        ug = P.split1(P.sb("ug", [64, 4, CH], BF16), CH)
        vg = P.sb("vg", [128, 16, 64], F32)
        xm = P.sb("xm", [128, 16, 64], F32)
        s1 = P.sb("s1", [128, 16], F32)
        s2 = P.sb("s2", [128, 16], F32)
        vnb = P.sb("vnb", [128, 16, 64], BF16)
        yb = P.sb("yb", [64, CH], F32)

        def proj(out_ps, col0, ncols, rhs3=None):
            rhs3 = rhs3 if rhs3 is not None else hT
            for k in range(8):
                P.mm(out_ps, Win[:, k, col0:col0 + ncols], rhs3[:, k, :], start=(k == 0), stop=(k == 7))

        def run_chains(chains):
            queue = list(chains)
            active = []
            free = list(range(NSLOT))
            while queue or active:
                while queue and len(free) >= queue[0][0]:
                    n, fac = queue.pop(0)
                    sl = [free.pop() for _ in range(n)]
                    active.append((fac(sl), sl))
                for item in list(active):
                    try:
                        next(item[0])
                    except StopIteration:
                        active.remove(item)
                        free.extend(item[1])

        def chain_norm(slots, src_fn, M, ones_lhsT, gain_col, out_bf, rope_out=None, after=None):
            sl = slots[0]
            ps_ = pa[sl]
            src_fn(ps_)
            yield
            q = qf[sl]
            P.copy("act", q[0:M, :], ps_[0:M, :])
            P.tt("pool" if sl % 2 == 0 else "dve", sqt[sl][0:M, :], q[0:M, :], q[0:M, :], ALU.mult)
            yield
            ms = pm[sl]
            P.mm(ms[0:M, :], ones_lhsT, sqt[sl][0:M, :])
            yield
            rs = rs_t[sl]
            self.rstd_from_ms(ms[0:M, :], M, rs[0:M, :], rs[0:M, :])
            dst = out_bf if rope_out is None else qn_t[sl][:]
            if gain_col is None:
                P.tt("dve", dst, q[0:M, :], rs[0:M, :], ALU.mult)
            else:
                P.stt(dst, q[0:M, :], gain_col, rs[0:M, :], ALU.mult, ALU.mult)
            if rope_out is not None:
                yield
                qn = qn_t[sl][:]
                pq = pm[sl]
                P.mm(pq[0:96, :], perm96, qn)
                t1 = rs_t[sl][0:96, :]
                t2 = qf[sl][0:96, :]
                P.tt("pool", t1, qn, Ctab[:], ALU.mult)
                yield
                P.tt("dve", t2, pq[0:96, :], Stab[:], ALU.mult)
                P.tt("pool" if sl % 2 == 1 else "dve", rope_out, t1, t2, ALU.add)
            if after is not None:
                after()

        for cg in range(NCH):
            b, c = divmod(cg, CPB)
            c0 = cg * CH
            cb0 = c * CH
            xcc = xc[cg % len(xc)]
            if l == 0:
                for tb in range(4):
                    xt_ = xtm[tb % 2]
                    P.dma("sp", xt_[:], I["x"][c0 + tb * 128:c0 + (tb + 1) * 128, :])
                    for k in range(0, 8, 4):
                        ps_ = pa[(2 * tb + k // 4) % NSLOT]
                        for kk in range(4):
                            P.mm(ps_[:, kk * 128:(kk + 1) * 128], xt_[:, (k + kk) * 128:(k + kk + 1) * 128], ident_f)
                        dst = xcc[:, k:k + 4, tb * 128:(tb + 1) * 128]
                        P.copy("act" if (k // 4 + tb) % 2 == 0 else "dve", dst,
                               ps_[:].rearrange("p (k t) -> p k t", k=4))
                P.dma("dq", S["xT"].rearrange("(k p) t -> p k t", p=128)[:, :, c0:c0 + CH], xcc[:],
                      wk=[("xT", cg)])
            else:
                P.dma("sp", xcc[:], S["xT"].rearrange("(k p) t -> p k t", p=128)[:, :, c0:c0 + CH],
                      rk=[("xT", cg)])
            ms = px
            for k in range(8):
                sq_ = sqx[k % 2]
                P.tt("dve" if k % 2 == 0 else "pool", sq_[:], xcc[:, k, :], xcc[:, k, :], ALU.mult)
                P.mm(ms[:], ones1024[:], sq_[:], start=(k == 0), stop=(k == 7))
            self.rstd_from_ms(ms[:], 128, rstd_x[:], rstd_x[:])
            for k in range(8):
                P.tt("dve" if k % 2 == 0 else "pool", hT[:, k, :], xcc[:, k, :], rstd_x[:], ALU.mult)
            P.dma("sp", posi[:], I["positions"][b:b + 1, cb0:cb0 + CH].partition_broadcast(96))
            P.copy("dve", ang[:], posi[:])
            P.ts("dve", ang[:], ang[:], cf[0:96, C_INV:C_INV + 1], ALU.mult)
            P.ts("dve", ang2[:], ang[:], float(np.pi / 2), ALU.add)
            for (a_, tab, sgn) in ((ang, Stab, True), (ang2, Ctab, False)):
                P.ts("dve", uu[:], a_[:], float(1.0 / (2 * np.pi)), ALU.mult)
                P.copy("dve", posi[:], uu[:])
                P.copy("dve", uu[:], posi[:])
                P.stt(a_[:], uu[:], -6.28125, a_[:], ALU.mult, ALU.add)
                P.stt(a_[:], uu[:], float(-(2 * np.pi - 6.28125)), a_[:], ALU.mult, ALU.add)
                P.ts("dve", a_[:], a_[:], 3.1415925, ALU.min, -3.1415925, ALU.max)
                P.act(tab[:], a_[:], AF.Sin)
                if sgn:
                    P.ts("dve", tab[:], tab[:], cf[0:96, C_SGN:C_SGN + 1], ALU.mult)
            def ch_forget(slots):
                sl = slots[0]
                pf_ = pa[sl]
                proj(pf_[0:12, :], OFF_F3, 12)
                yield
                P.act(ef[:], pf_[0:12, :], AF.Exp, bias=nbf[:], scale=-1.0)
                P.act(ef[:], ef[:], AF.Ln, bias=1.0)
                Fcur = Fs[cg % 2]
                init = 0.0 if c == 0 else Fs[(cg - 1) % 2][:, CH - 1:CH]
                P.scan(Fcur[:], onesf[:], ef[:], init, ALU.mult, ALU.subtract)
                yield
                P.copy("dve", hi[:], Fcur[:])
                P.tt("dve", r1[:], Fcur[:], hi[:], ALU.subtract)
                P.copy("dve", lo[:], r1[:])
                P.tt("dve", r1[:], r1[:], lo[:], ALU.subtract)
                P.copy("dve", lo2[:], r1[:])
                P.ts("dve", c1[:], hi[:], m3[:, 0:1], ALU.mult)
                P.stt(c1[:], lo[:], m3[:, 1:2], c1[:], ALU.mult, ALU.add)
                P.stt(Faug[0:12, :], lo2[:], m3[:, 2:3], c1[:], ALU.mult, ALU.add)
                yield
                i = 0
                for h in range(4):
                    for (dst, selc) in ((qaugA, C_SELQ), (kaugA, C_SELK)):
                        pss = pm[sl] if i % 2 == 0 else pa[sl]
                        i += 1
                        P.mm(pss[0:70, :], cb[0:13, selc + h * 70:selc + (h + 1) * 70], Faug[:])
                        yield
                        P.copy("act", dst[64:70, h, :], pss[64:70, :])

            def ch_av(slots):
                for tb in range(4):
                    for k in range(8):
                        P.mm(pv[:, tb * 256:(tb + 1) * 256], hT[:, k, tb * 128:(tb + 1) * 128],
                             Win[:, k, OFF_AV:OFF_AV + 256], start=(k == 0), stop=(k == 7))
                    if tb % 2 == 1:
                        yield
                for tb in range(4):
                    P.copy("act" if tb % 2 == 0 else "dve", vAt[:, :, tb, 0:64],
                           pv[:, tb * 256:(tb + 1) * 256].rearrange("p (h d) -> p h d", h=4))
                yield

            def ch_bu(slots, g_):
                sl = slots[0]
                ps_ = pa[sl]
                proj(ps_[0:64, :], OFF_BU + g_ * 64, 64)
                yield
                P.act(ug[:, g_, :], ps_[0:64, :], AF.Gelu)

            def ch_bv(slots):
                for tb in range(4):
                    for k in range(8):
                        P.mm(pv[:, tb * 256:(tb + 1) * 256], hT[:, k, tb * 128:(tb + 1) * 128],
                             Win[:, k, OFF_BV:OFF_BV + 256], start=(k == 0), stop=(k == 7))
                    if tb % 2 == 1:
                        yield
                P.act(vg[:].rearrange("p a d -> p (a d)"), pv[:], AF.Gelu)
                P.reduce("dve", s1[:], vg[:], ALU.add)
                P.ts("dve", s1[:], s1[:], 1.0 / 64, ALU.mult)
                yield
                P.tt("dve", xm[:], vg[:], s1[:].unsqueeze(2).to_broadcast([128, 16, 64]), ALU.subtract)
                P.tt("pool", vg[:], xm[:], xm[:], ALU.mult)
                yield
                P.reduce("dve", s2[:], vg[:], ALU.add)
                P.act(s2[:], s2[:], AF.Ln, bias=self.eps_col[:, 0:1], scale=1.0 / 64)
                P.act(s2[:], s2[:], AF.Exp, scale=-0.5)
                yield
                P.tt("dve", xm[:], xm[:], s2[:].unsqueeze(2).to_broadcast([128, 16, 64]), ALU.mult)
                P.tt("pool", vnb[:].rearrange("p (a g) d -> p a (g d)", a=4), xm[:].rearrange("p (a g) d -> p a (g d)", a=4),
                     bvg[:].unsqueeze(1).to_broadcast([128, 4, 256]), ALU.mult)
                yield
                sl = slots[0]
                for g_ in range(4):
                    py = pm[sl] if g_ % 2 == 0 else pa[sl]
                    for tb in range(4):
                        P.mm(py[0:64, tb * 128:(tb + 1) * 128], vnb[:, tb * 4 + g_, :], wmT[:, g_, :])
                    yield
                    P.tt("dve", yb[:].rearrange("p (a t) -> p a t", a=4), py[0:64, :].rearrange("p (a t) -> p a t", a=4),
                         bsb[:, g_, :].unsqueeze(1).to_broadcast([64, 4, 128]), ALU.add)
                    P.tt("pool", oBt[:, g_, :], yb[:], ug[:, g_, :], ALU.mult)

            def ch_cq(slots):
                s0, s1_ = slots
                proj(pa[s0][:], OFF_CQ, 128)
                proj(pa[s1_][:], OFF_CQ + 128, 128)
                yield
                P.copy("act", qf[s0][:], pa[s0][:])
                P.copy("act", qf[s1_][:], pa[s1_][:])
                P.tt("pool", sqt[s0][:], qf[s0][:], qf[s0][:], ALU.mult)
                P.tt("pool", sqt[s1_][:], qf[s1_][:], qf[s1_][:], ALU.mult)
                yield
                ms = pm[s0]
                P.mm(ms[:], ones256[:], sqt[s0][:], start=True, stop=False)
                P.mm(ms[:], ones256[:], sqt[s1_][:], start=False, stop=True)
                yield
                self.rstd_from_ms(ms[:], 128, rs_t[s0][:], rs_t[s0][:])
                P.tt("dve", cqn[:, 0, :], qf[s0][:], rs_t[s0][:], ALU.mult)
                P.tt("dve", cqn[:, 1, :], qf[s1_][:], rs_t[s0][:], ALU.mult)

            def src_q(h):
                def f(ps_):
                    for m in range(2):
                        P.mm(ps_[0:96, :], Wuq[:, m, h * 96:(h + 1) * 96], cqn[:, m, :], start=(m == 0), stop=(m == 1))
                return f

            def src_k(h):
                return lambda ps_: P.mm(ps_[0:64, :], Wukv[:, h * 64:(h + 1) * 64], ckvn[:])

            def ch_cv(slots):
                for tb in range(4):
                    half = pv[:, (tb % 2) * 512:(tb % 2 + 1) * 512]
                    P.mm(half, ckvn[:, tb * 128:(tb + 1) * 128], Wukv[:, 512:1024])
                    yield
                    P.copy("act" if tb % 2 == 0 else "dve", vCt[:, :, tb, 0:64],
                           half.rearrange("p (h d) -> p h d", h=8))

            chains = [(1, ch_forget), (2, ch_cq),
                      (1, lambda sl: chain_norm(sl, lambda ps_: proj(ps_[:], OFF_CKV, 128), 128, ones128[:], None, ckvn[:])),
                      (1, lambda sl: chain_norm(sl, lambda ps_: proj(ps_[0:96, :], OFF_KRP, 96), 96, bd96, g96kr, None,
                                                rope_out=krf[:]))]
            for h in range(4):
                chains.append((1, lambda sl, h=h: chain_norm(sl, lambda ps_: proj(ps_[0:64, :], OFF_AQ + h * 64, 64), 64,
                                                             ones64[:], gq_a[:], qaugA[0:64, h, :])))
                chains.append((1, lambda sl, h=h: chain_norm(sl, lambda ps_: proj(ps_[0:64, :], OFF_AK + h * 64, 64), 64,
                                                             ones64[:], gk_a, kaugA[0:64, h, :])))
            chains.append((0, ch_av))
            for g_ in range(4):
                chains.append((1, lambda sl, g_=g_: ch_bu(sl, g_)))
            chains.append((1, ch_bv))
            for h in range(8):
                chains.append((1, lambda sl, h=h: chain_norm(sl, src_q(h), 96, bd96, g96q[:], None, rope_out=qCt[:, h, :])))
                chains.append((1, lambda sl, h=h: chain_norm(
                    sl, src_k(h), 64, ones64[:], gkn, kCt[0:64, h, :],
                    after=(lambda: P.copy("act" if h % 2 == 0 else "pool", kCt[64:96, h, :], krf[64:96, :])))))
                if h == 3:
                    chains.append((0, ch_cv))
            run_chains(chains)
            P.dma("dq", S["qA"][b, :, :, cb0:cb0 + CH], qaugA[:])
            P.dma("dq", S["kA"][b, :, :, cb0:cb0 + CH], kaugA[:])
            P.dma("dq", S["qC"][b, :, :, cb0:cb0 + CH], qCt[:])
            P.dma("dq", S["kC"][b, :, :, cb0:cb0 + CH], kCt[:])
            P.dma("dq", S["vA"][b].rearrange("p h (n d) -> p h n d", d=65)[:, :, 4 * c:4 * c + 4, :], vAt[:])
            P.dma("dq", S["vC"][b].rearrange("p h (n d) -> p h n d", d=65)[:, :, 4 * c:4 * c + 4, :], vCt[:])
            P.dma("dq", S["oT"][256:512, :].rearrange("(g p) t -> p g t", p=64)[:, :, c0:c0 + CH], oBt[:],
                  wk=[("oT", cg)])

    def phase_attn(self, l):
        P, I, S = self.P, self.I, self.S
        cf, cb = self.load_consts()
        maskA = cb[:, C_MASKA:C_MASKA + 128]
        maskC = cb[:, C_MASKC:C_MASKC + 128]
        sel65 = cf[0:65, C_SEL65:C_SEL65 + 64]
        qt = [P.sb(f"qt{i}", [96, SEQ], BF16) for i in range(2)]
        kt = [P.sb(f"kt{i}", [96, SEQ], BF16) for i in range(2)]
        vt = [P.sb(f"vt{i}", [128, 32, 65], BF16) for i in range(2)]
        NST = 5
        pst_ = [P.ps(f"st{i}") for i in range(NST)]
        po = [P.ps("po0"), P.ps("po1")]
        prs = P.ps("prs")
        PT = [P.sb(f"PT{i}", [128, CH], BF16) for i in range(NST)]
        of = [P.sb(f"of{i}", [65, CH], F32) for i in range(2)]
        rinv = [P.sb(f"rinv{i}", [64, CH], F32) for i in range(2)]
        ob = [P.sb(f"ob{i}", [64, CH], BF16) for i in range(2)]
        hidx = 0
        for b in range(NBC):
            for (kind, nh, dk, qs, ks, vs, mask, row0) in (("A", 4, 70, "qA", "kA", "vA", maskA, 0),
                                                           ("C", 8, 96, "qC", "kC", "vC", maskC, 512)):
                for h in range(nh):
                    q_, k_, v_ = qt[hidx % 2], kt[hidx % 2], vt[hidx % 2]
                    hidx += 1
                    P.dma("sp", q_[0:dk, :], S[qs][b, :, h, :])
                    P.dma("sp", k_[0:dk, :], S[ks][b, :, h, :])
                    P.dma("sp", v_[:].rearrange("p n d -> p (n d)"), S[vs][b, :, h, :])
                    pairs = []
                    for c in range(CPB):
                        for j in range(4 * c + 4):
                            pairs.append((c, j))
                    LOOK = 3
                    npair = len(pairs)

                    def score(i):
                        c, j = pairs[i]
                        r = j - 4 * c
                        q0 = c * CH + (128 * r if r > 0 else 0)
                        n = CH - (128 * r if r > 0 else 0)
                        st_ = pst_[i % NST]
                        P.mm(st_[:, 0:n], k_[0:dk, j * 128:(j + 1) * 128], q_[0:dk, q0:q0 + n])
                        pt_ = PT[i % NST]
                        P.act(pt_[:, 0:n], st_[:, 0:n], AF.Exp)
                        if r >= 0:
                            P.tt("dve", pt_[:, 0:128], pt_[:, 0:128], mask, ALU.mult)

                    def pv_(i):
                        c, j = pairs[i]
                        r = j - 4 * c
                        off = (128 * r if r > 0 else 0)
                        n = CH - off
                        o_ = po[c % 2]
                        last = (j == 4 * c + 3)
                        P.mm(o_[0:65, off:CH], v_[:, j, :], PT[i % NST][:, 0:n], start=(j == 0), stop=last)
                        if last:
                            of_ = of[c % 2]
                            P.copy("dve", of_[:], o_[0:65, :])

                            def fin(c=c, of_=of_):
                                P.mm(prs[0:64, :], sel65, of_[:])
                                ri = rinv[c % 2]
                                P.recip(ri[:], prs[0:64, :])
                                ob_ = ob[c % 2]
                                P.tt("dve", ob_[:], of_[0:64, :], ri[:], ALU.mult)
                                cg = b * CPB + c
                                r0 = row0 + h * 64
                                P.dma("dq", S["oT"][r0:r0 + 64, cg * CH:(cg + 1) * CH], ob_[:])
                            deferred.append([3, fin])

                    deferred = []
                    for i in range(npair + LOOK):
                        if i < npair:
                            score(i)
                        if i >= LOOK:
                            pv_(i - LOOK)
                        for d_ in list(deferred):
                            d_[0] -= 1
                            if d_[0] <= 0:
                                d_[1]()
                                deferred.remove(d_)
                    for d_ in deferred:
                        d_[1]()

    def phase_mix_out(self, l):
        P, I, S = self.P, self.I, self.S
        cf, cb = self.load_consts()
        ident_f = cf[:, C_ID:C_ID + 128]
        self.eps_col = self.const_tile("epsc", [128, 1], EPS, F32)
        ones1024 = self.const_tile("on1024", [128, 128], 1.0 / 1024)
        ones512 = self.const_tile("on512", [128, 128], 1.0 / 512)
        ones256 = self.const_tile("on256", [128, 128], 1.0 / 256)
        ones128 = self.const_tile("on128", [128, 128], 1.0 / 128)
        ones1 = self.const_tile("on1", [128, 128], 1.0)
        Wout = P.sb("Wout", [128, 8, 1024], BF16)
        P.dma("sp", Wout[:], S[f"Wout{l}"].rearrange("k p n -> p k n"))
        Wmq = P.sb("Wmq", [128, 8, 512], BF16)
        P.dma("sp", Wmq[:], S[f"Wmq{l}"].rearrange("k p n -> p k n"))
        Wmo = P.sb("Wmo", [128, 4, 1024], BF16)
        P.dma("sp", Wmo[:], S[f"Wmo{l}"].rearrange("k p n -> p k n"))
        Wmkv = P.sb("Wmkv", [128, 8, 1024], BF16)
        P.dma("sp", Wmkv[:], S[f"Wmkv{l}"].rearrange("k p n -> p k n"))
        prm = P.sb("prm", [128, 4], F32)
        P.dma("sp", prm[:, 0:1], col1(I["m_q_norm"][l]))
        P.dma("sp", prm[:, 1:2], col1(I["m_k_norm"][l]))
        gmq = P.sb("gmq", [128, 1], F32)
        P.ts("dve", gmq[:], prm[:, 0:1], 128.0 ** -0.5, ALU.mult)
        gmk = prm[:, 1:2]
        moe = (l % 2 == 1)
        if moe:
            Wr = P.sb("Wr", [128, 8, 8], F32)
            P.dma("sp", Wr[:], I["w_router"][0].rearrange("(k p) e -> p k e", p=128))
            gf = P.sb("gf", [128, 8], F32)
            for k in range(8):
                P.dma("sp", gf[:, k:k + 1], col1(I["ffn_norm"][l, k * 128:(k + 1) * 128]))
            for k in range(8):
                P.ts("dve", Wr[:, k, :], Wr[:, k, :], gf[:, k:k + 1], ALU.mult)
            brt = P.sb("brt", [128, 8], F32)
            P.dma("sp", brt[:], I["b_router"][0:1, :].partition_broadcast(128))
        bk = [P.ps(f"bk{i}") for i in range(8)]
        self._i = {}

        def nxt(lst, key):
            i = self._i.get(key, 0)
            self._i[key] = i + 1
            return lst[i % len(lst)]

        def nb():
            return nxt(bk, "bk")

        xc = [P.sb(f"xc{i}", [128, 8, CH], F32) for i in range(1 if moe else 2)]
        oc = [P.sb(f"oc{i}", [128, 8, CH], BF16) for i in range(2)]
        sqx = [P.sb(f"sqx{i}", [128, CH], BF16) for i in range(4)]
        on = P.split1(P.sb("on", [128, 8, CH], BF16), CH)
        h2T = P.split1(P.sb("h2T", [128, 8, CH], BF16), CH)
        h3T = P.sb("h3T", [128, 8, CH], BF16)
        h3f = P.sb("h3f", [128, 8, CH], F32) if moe else None
        rstd = [P.sb(f"rstd{i}", [128, CH], F32) for i in range(3)]
        qfh = [P.sb(f"qfh{i}", [128, CH], F32) for i in range(4)]
        sqh = [P.sb(f"sqh{i}", [128, CH], BF16) for i in range(4)]
        rsh = [P.sb(f"rsh{i}", [128, CH], F32) for i in range(4)]
        qnh = [P.sb(f"qnh{i}", [128, CH], BF16) for i in range(4)]
        PTh = [[P.sb(f"PT{i}_{j}", [128, CH], BF16) for j in range(2)] for i in range(4)]
        om = P.split1(P.sb("om", [128, 4, CH], BF16), CH)
        KmT = P.sb("KmT", [128, 4, MEM], BF16)
        Vm = P.sb("Vm", [128, 2, 512], BF16)
        memt = P.sb("memt", [128, D], F32)
        memT = P.sb("memT", [128, 8, MEM], F32)
        msq = P.sb("msq", [128, 8, MEM], BF16)
        mnT = P.sb("mnT", [128, 8, MEM], BF16)
        if moe:
            lg = P.sb("lg", [128, 4, 8], F32)
            eq1 = P.sb("eq1", [128, 4, 8], F32)
            eq2 = P.sb("eq2", [128, 4, 8], F32)
            l2 = P.sb("l2", [128, 4, 8], F32)
            sc = P.sb("sc", [128, 8, 4], F32)
            comb = P.sb("comb", [128, 4, 8], F32)
            cTt = P.sb("cTt", [8, CH], F32)

        def ms_accum(src3, nk, ones_lhsT, bank, n=CH):
            for k in range(nk):
                sq_ = nxt(sqx, "sqx")
                P.tt("dve" if k % 2 == 0 else "pool", sq_[:, 0:n], src3[:, k, :], src3[:, k, :], ALU.mult)
                P.mm(bank[:, 0:n], ones_lhsT, sq_[:, 0:n], start=(k == 0), stop=(k == nk - 1))

        def rmsT(src3, ones_lhsT, out3, nk, out_f32=None):
            n = src3.shape[2]
            ms = nb()
            ms_accum(src3, nk, ones_lhsT, ms, n)
            rs = nxt(rstd, "rstd")
            self.rstd_from_ms(ms[:, 0:n], 128, rs[:, 0:n], rs[:, 0:n])
            for k in range(nk):
                P.tt("dve" if k % 2 == 0 else "pool", out3[:, k, :], src3[:, k, :], rs[:, 0:n], ALU.mult)
                if out_f32 is not None:
                    P.tt("pool" if k % 2 == 0 else "dve", out_f32[:, k, :], src3[:, k, :], rs[:, 0:n], ALU.mult)

        def run_rr(gens):
            active = list(gens)
            while active:
                for g in list(active):
                    try:
                        next(g)
                    except StopIteration:
                        active.remove(g)

        for cg in range(NCH):
            b, c = divmod(cg, CPB)
            c0 = cg * CH
            if c == 0:
                for mb in range(2):
                    P.dma("sp", memt[:], I["mem"][b * MEM + mb * 128:b * MEM + (mb + 1) * 128, :])
                    for k in range(0, 8, 4):
                        ps_ = nb()
                        for kk in range(4):
                            P.mm(ps_[:, kk * 128:(kk + 1) * 128], memt[:, (k + kk) * 128:(k + kk + 1) * 128], ident_f)
                        P.copy("act", memT[:, k:k + 4, mb * 128:(mb + 1) * 128], ps_[:].rearrange("p (k t) -> p k t", k=4))
                P.tt("pool", msq[:], memT[:], memT[:], ALU.mult)
                ms = nb()
                for k in range(8):
                    P.mm(ms[:, 0:MEM], ones1024[:], msq[:, k, :], start=(k == 0), stop=(k == 7))
                rs = nxt(rstd, "rstd")
                self.rstd_from_ms(ms[:, 0:MEM], 128, rs[:, 0:MEM], rs[:, 0:MEM])
                for k in range(8):
                    P.tt("dve", mnT[:, k, :], memT[:, k, :], rs[:, 0:MEM], ALU.mult)
                for h in range(4):
                    ps_ = nb()
                    for k in range(8):
                        P.mm(ps_[:, 0:MEM], Wmkv[:, k, h * 128:(h + 1) * 128], mnT[:, k, :], start=(k == 0), stop=(k == 7))
                    qf_ = qfh[h]
                    P.copy("act", qf_[:, 0:MEM], ps_[:, 0:MEM])
                    sq_ = sqh[h]
                    P.tt("pool", sq_[:, 0:MEM], qf_[:, 0:MEM], qf_[:, 0:MEM], ALU.mult)
                    ms = nb()
                    P.mm(ms[:, 0:MEM], ones128[:], sq_[:, 0:MEM])
                    rs = rsh[h]
                    self.rstd_from_ms(ms[:, 0:MEM], 128, rs[:, 0:MEM], rs[:, 0:MEM])
                    P.stt(KmT[:, h, :], qf_[:, 0:MEM], gmk, rs[:, 0:MEM], ALU.mult, ALU.mult)
                for mb in range(2):
                    ps_ = nb()
                    for k in range(8):
                        P.mm(ps_[:], mnT[:, k, mb * 128:(mb + 1) * 128], Wmkv[:, k, 512:1024], start=(k == 0), stop=(k == 7))
                    P.copy("act", Vm[:, mb, :], ps_[:])
            xcc = xc[cg % len(xc)]
            occ = oc[cg % 2]
            P.dma("sp", occ[:], S["oT"].rearrange("(k p) t -> p k t", p=128)[:, :, c0:c0 + CH])
            P.dma("sp", xcc[:], S["xT"].rearrange("(k p) t -> p k t", p=128)[:, :, c0:c0 + CH])
            groups = ((0, 2, ones256), (2, 2, ones256), (4, 4, ones512))
            gms = []
            for (k0, nk, ones_) in groups:
                ms = nb()
                ms_accum(occ[:, k0:k0 + nk, :], nk, ones_[:], ms)
                gms.append(ms)
            grs = []
            for gi in range(3):
                rs = nxt(rstd, "rstd")
                self.rstd_from_ms(gms[gi][:], 128, rs[:], rs[:])
                grs.append(rs)
            for gi, (k0, nk, ones_) in enumerate(groups):
                for k in range(nk):
                    P.tt("dve" if k % 2 == 0 else "pool", on[:, k0 + k, :], occ[:, k0 + k, :], grs[gi][:], ALU.mult)
            for m in range(8):
                ps_ = nb()
                for k in range(8):
                    P.mm(ps_[:], Wout[:, k, m * 128:(m + 1) * 128], on[:, k, :], start=(k == 0), stop=(k == 7))
                P.tt("dve", xcc[:, m, :], xcc[:, m, :], ps_[:], ALU.add)
            rmsT(xcc[:], ones1024[:], h2T, 8)

            def head(h):
                b0, b1 = bk[2 * h], bk[2 * h + 1]
                for k in range(8):
                    P.mm(b0[:], Wmq[:, k, h * 128:(h + 1) * 128], h2T[:, k, :], start=(k == 0), stop=(k == 7))
                yield
                qf_ = qfh[h]
                P.copy("act", qf_[:], b0[:])
                P.tt("pool" if h % 2 == 0 else "dve", sqh[h][:], qf_[:], qf_[:], ALU.mult)
                yield
                P.mm(b1[:], ones128[:], sqh[h][:])
                yield
                rs = rsh[h]
                self.rstd_from_ms(b1[:], 128, rs[:], rs[:])
                qn = qnh[h]
                P.stt(qn[:], qf_[:], gmq[:], rs[:], ALU.mult, ALU.mult)
                yield
                P.mm(b0[:], KmT[:, h, 0:128], qn[:])
                P.mm(b1[:], KmT[:, h, 128:256], qn[:])
                yield
                P.act(PTh[h][0][:], b0[:], AF.Exp)
                P.act(PTh[h][1][:], b1[:], AF.Exp)
                yield
                for mb in range(2):
                    P.mm(b0[:], Vm[:, mb, h * 128:(h + 1) * 128], PTh[h][mb][:], start=(mb == 0), stop=(mb == 1))
                for mb in range(2):
                    P.mm(b1[:], ones1[:], PTh[h][mb][:], start=(mb == 0), stop=(mb == 1))
                yield
                ri = qfh[h]
                P.recip(ri[:], b1[:])
                P.tt("dve", om[:, h, :], b0[:], ri[:], ALU.mult)

            run_rr([head(h) for h in range(4)])
            for m in range(8):
                ps_ = nb()
                for h in range(4):
                    P.mm(ps_[:], Wmo[:, h, m * 128:(m + 1) * 128], om[:, h, :], start=(h == 0), stop=(h == 3))
                P.tt("dve", xcc[:, m, :], xcc[:, m, :], ps_[:], ALU.add)
            P.dma("dq", S["xT"].rearrange("(k p) t -> p k t", p=128)[:, :, c0:c0 + CH], xcc[:])
            rmsT(xcc[:], ones1024[:], h3T, 8, out_f32=h3f)
            P.dma("dq", S["hT3"].rearrange("(k p) t -> p k t", p=128)[:, :, c0:c0 + CH], h3T[:])
            if moe:
                pl = nb()
                for tb in range(4):
                    for k in range(8):
                        P.mm(pl[:, tb * 8:(tb + 1) * 8], h3f[:, k, tb * 128:(tb + 1) * 128], Wr[:, k, :],
                             start=(k == 0), stop=(k == 7))
                B3 = [128, 4, 8]
                P.tt("dve", lg[:], pl[:, 0:32].rearrange("p (a e) -> p a e", a=4), brt[:].unsqueeze(1).to_broadcast(B3), ALU.add)
                m1, m2, dd, ee, den, g1, g2 = (sc[:, i, :] for i in range(7))
                P.reduce("dve", m1, lg[:], ALU.max)
                P.tt("dve", eq1[:], lg[:], m1.unsqueeze(2).to_broadcast(B3), ALU.is_equal)
                P.stt(l2[:], eq1[:], -1e30, lg[:], ALU.mult, ALU.add)
                P.reduce("dve", m2, l2[:], ALU.max)
                P.tt("dve", eq2[:], l2[:], m2.unsqueeze(2).to_broadcast(B3), ALU.is_equal)
                P.tt("dve", dd, m2, m1, ALU.subtract)
                P.act(ee, dd, AF.Exp)
                P.ts("dve", den, ee, 1.0, ALU.add)
                P.recip(g1, den)
                P.tt("dve", g2, ee, g1, ALU.mult)
                P.tt("dve", comb[:], eq1[:], g1.unsqueeze(2).to_broadcast(B3), ALU.mult)
                P.tt("dve", eq2[:], eq2[:], g2.unsqueeze(2).to_broadcast(B3), ALU.mult)
                P.tt("dve", comb[:], comb[:], eq2[:], ALU.add)
                pc = nb()
                for tb in range(4):
                    P.mm(pc[0:8, tb * 128:(tb + 1) * 128], comb[:, tb, :], ident_f)
                P.copy("act", cTt[:], pc[0:8, :])
                P.dma("dq", S["combT"][:, c0:c0 + CH], cTt[:])

    def phase_ffn_all(self, l):
        P, I, S = self.P, self.I, self.S
        moe = (l % 2 == 1)
        if not moe:
            passes = [(I["ffn_w_gate"][l // 2], I["ffn_w_up"][l // 2], I["ffn_w_down"][l // 2], 0, 1408, None),
                      (I["ffn_w_gate"][l // 2], I["ffn_w_up"][l // 2], I["ffn_w_down"][l // 2], 1408, 1344, None)]
        else:
            passes = [(I["moe_w_gate"][l // 2, e], I["moe_w_up"][l // 2, e], I["moe_w_down"][l // 2, e], 0, 1408, e)
                      for e in range(NE)]
        NH, NJ = 1408, 11
        Wg = [P.sb(f"Wg{i}", [128, 8, NH], BF16) for i in range(2)]
        Wu = [P.sb(f"Wu{i}", [128, 8, NH], BF16) for i in range(2)]
        Wd = [P.sb(f"Wd{i}", [128, NJ, D], BF16) for i in range(2)]
        stg = [P.sb(f"stg{i}", [128, NH], F32) for i in range(2)]
        gcol = P.sb("gcol", [128, 8], F32)
        for k in range(8):
            P.dma("sp", gcol[:, k:k + 1], col1(I["ffn_norm"][l, k * 128:(k + 1) * 128]))
        if moe:
            selt = P.sb("selt", [8, NE * 128], F32)
            P.dma("sp", selt[:], I["cst"][0:8, C_SELE:C_SELE + NE * 128])
            cTt = [P.sb(f"cTt{i}", [8, CH], F32) for i in range(2)]
            cbt = [P.sb(f"cbt{i}", [128, CH], F32) for i in range(2)]
            pcb = P.ps("pcb")
        pg = [P.ps("pg0"), P.ps("pg1")]
        pu = [P.ps("pu0"), P.ps("pu1")]
        py = [P.ps("py0"), P.ps("py1")]
        xc = [P.sb(f"xc{i}", [128, 8, CH], F32) for i in range(1)]
        hc = [P.sb(f"hc{i}", [128, 8, CH], BF16) for i in range(2)]
        at = [P.split1(P.sb(f"at{i}", [128, NJ, CH], BF16), CH) for i in range(1)]
        sg = [P.sb(f"sg{i}", [128, CH], F32) for i in range(2)]
        xTv = S["xT"].rearrange("(k p) t -> p k t", p=128)
        hTv = S["hT3"].rearrange("(k p) t -> p k t", p=128)
        self._stg_i = 0

        def prep_tasks(pi):
            wg_, wu_, wd_, hid0, nhid, _ = passes[pi]
            bi = pi % 2
            for (src, dst) in ((wg_, Wg[bi]), (wu_, Wu[bi])):
                for k in range(8):
                    st_ = stg[self._stg_i % 2]
                    self._stg_i += 1
                    P.dma("sp", st_[:, 0:nhid], src[k * 128:(k + 1) * 128, hid0:hid0 + nhid])
                    if self._stg_i % 2 == 0:
                        P.ts("dve", dst[:, k, 0:nhid], st_[:, 0:nhid], gcol[:, k:k + 1], ALU.mult)
                    else:
                        P.act(dst[:, k, 0:nhid], st_[:, 0:nhid], AF.Copy, scale=gcol[:, k:k + 1])
                    yield
            nj = (nhid + 127) // 128
            for j in range(nj):
                rows = min(128, nhid - j * 128)
                st_ = stg[self._stg_i % 2]
                self._stg_i += 1
                P.dma("sp", st_[0:rows, 0:D], wd_[hid0 + j * 128:hid0 + j * 128 + rows, :])
                P.copy("act" if self._stg_i % 2 == 0 else "dve", Wd[bi][0:rows, j, :], st_[0:rows, 0:D])
                yield

        for _ in prep_tasks(0):
            pass
        n = 0
        for pi, (wg_, wu_, wd_, hid0, nhid, expert) in enumerate(passes):
            bi = pi % 2
            nj = (nhid + 127) // 128
            nxt_prep = prep_tasks(pi + 1) if pi + 1 < len(passes) else iter(())
            for cg in range(NCH):
                c0 = cg * CH
                xcc, hcc, a_ = xc[0], hc[cg % 2], at[0]
                if expert is not None:
                    ct = cTt[cg % 2]
                    P.dma("sp", ct[:], S["combT"][:, c0:c0 + CH])
                P.dma("sp", hcc[:], hTv[:, :, c0:c0 + CH])
                P.dma("sp", xcc[:], xTv[:, :, c0:c0 + CH])
                if expert is not None:
                    P.mm(pcb[:], selt[:, expert * 128:(expert + 1) * 128], ct[:])
                    cbc = cbt[cg % 2]
                    P.copy("act", cbc[:], pcb[:])
                for j in range(nj):
                    rows = min(128, nhid - j * 128)
                    g_, u_ = pg[j % 2], pu[j % 2]
                    for k in range(8):
                        P.mm(g_[0:rows, :], Wg[bi][:, k, j * 128:j * 128 + rows], hcc[:, k, :], start=(k == 0), stop=(k == 7))
                    for k in range(8):
                        P.mm(u_[0:rows, :], Wu[bi][:, k, j * 128:j * 128 + rows], hcc[:, k, :], start=(k == 0), stop=(k == 7))
                    s_ = sg[n % 2]
                    n += 1
                    P.act(s_[0:rows, :], g_[0:rows, :], AF.Silu)
                    if expert is None:
                        P.tt("dve", a_[0:rows, j, :], s_[0:rows, :], u_[0:rows, :], ALU.mult)
                    else:
                        P.tt("dve", s_[0:rows, :], s_[0:rows, :], u_[0:rows, :], ALU.mult)
                        P.tt("pool", a_[0:rows, j, :], s_[0:rows, :], cbc[0:rows, :], ALU.mult)
                for m in range(8):
                    y_ = py[m % 2]
                    for j in range(nj):
                        rows = min(128, nhid - j * 128)
                        P.mm(y_[:], Wd[bi][0:rows, j, m * 128:(m + 1) * 128], a_[0:rows, j, :],
                             start=(j == 0), stop=(j == nj - 1))
                    P.tt("dve", xcc[:, m, :], xcc[:, m, :], y_[:], ALU.add)
                P.dma("dq", xTv[:, :, c0:c0 + CH], xcc[:])
                for _ in range(2):
                    next(nxt_prep, None)
            for _ in nxt_prep:
                pass

    def phase_final(self):
        P, I, S = self.P, self.I, self.S
        cf = P.sb("cstf", [128, CW], F32)
        P.dma("sp", cf[:], I["cst"])
        ident_f = cf[:, C_ID:C_ID + 128]
        xc = [P.sb(f"xc{i}", [128, 8, CH], F32) for i in range(2)]
        ot = [P.sb(f"ot{i}", [128, D], F32) for i in range(3)]
        pp = [P.ps(f"pp{i}", (128, 1024)) for i in range(3)]
        xTv = S["xT"].rearrange("(k p) t -> p k t", p=128)
        n = 0
        for cg in range(NCH):
            c0 = cg * CH
            xcc = xc[cg % 2]
            P.dma("sp", xcc[:], xTv[:, :, c0:c0 + CH], rk=[("xT", cg)])
            for tb in range(4):
                p_ = pp[n % 3]
                o_ = ot[n % 3]
                n += 1
                for k in range(8):
                    P.mm(p_[:, k * 128:(k + 1) * 128], xcc[:, k, tb * 128:(tb + 1) * 128], ident_f)
                P.copy("act", o_[:, 0:512], p_[:, 0:512])
                P.copy("dve", o_[:, 512:1024], p_[:, 512:1024])
                self.final_ops.append(P.dma("dq", self.out[c0 + tb * 128:c0 + (tb + 1) * 128, :], o_[:]))


_CACHE = {}


def _get_builder():
    if "b" not in _CACHE:
        b = Builder()
        b.build()
        _CACHE["b"] = b
    return _CACHE["b"]


def make_in_maps(inputs, n_cores=8):
    cst = build_consts()
    x = np.ascontiguousarray(inputs["x"], dtype=np.float32)
    mem = np.ascontiguousarray(inputs["mem"], dtype=np.float32)
    pos = np.ascontiguousarray(inputs["positions"], dtype=np.int32)
    shared = {"cst": cst}
    for name, shp in PARAM_SPECS:
        shared[name] = np.ascontiguousarray(inputs[name], dtype=np.float32).reshape(shp)
    maps = []
    for i in range(n_cores):
        m = dict(shared)
        m["x"] = x[NBC * i:NBC * (i + 1)].reshape(T, D)
        m["mem"] = mem[NBC * i:NBC * (i + 1)].reshape(NBC * MEM, D)
        m["positions"] = pos[NBC * i:NBC * (i + 1)]
        maps.append(m)
    return maps


def kernel(**inputs):
    b = _get_builder()
    maps = make_in_maps(inputs)
    res = run_bass_kernel_spmd(b.nc, maps, core_ids=list(range(8)))
    outs = [np.asarray(r["out"]).reshape(NBC, SEQ, D) for r in res.results]
    return np.concatenate(outs, axis=0).astype(np.float32)
```

```python
import contextlib
import numpy as np
import concourse.bass as bass
import concourse.mybir as mybir
from concourse.bass_utils import run_bass_kernel_spmd

F32 = mybir.dt.float32
BF16 = mybir.dt.bfloat16
I32 = mybir.dt.int32
ALU = mybir.AluOpType
AF = mybir.ActivationFunctionType
AX = mybir.AxisListType

COMPUTE = ("pe", "act", "dve", "pool")
DMAQ = ("sp", "dq")
STREAMS = COMPUTE + DMAQ
EPOCH = 12000
NDSEM = {"sp": 24, "dq": 12}


class Op:
    __slots__ = ("eng", "fn", "rk", "wk", "deps", "idx", "seq", "sig", "is_dma", "waits", "dslot")

    def __init__(self, eng, fn, rk, wk, is_dma):
        self.eng = eng
        self.fn = fn
        self.rk = rk
        self.wk = wk
        self.is_dma = is_dma
        self.deps = set()
        self.sig = None
        self.waits = []


def _key(ap):
    return ap.tensor.name


def _is_dram(ap):
    return type(ap.tensor).__name__.startswith("DRam")


class Prog:
    def __init__(self, nc):
        self.nc = nc
        self.ops = []
        self.pending = {s: set() for s in STREAMS}
        self.last_on = {}
        self.dma_since_bar = []
        self.phase_stack = None
        self.nphase = 0
        self.bar_aps = None
        self.split = {}

    def split1(self, t, stride):
        self.split[t.name] = stride
        return t

    def _keys(self, ap):
        name = ap.tensor.name
        st = self.split.get(name)
        if st is None:
            return [name]
        dims = ap.ap
        foff = ap.offset % dims[0][0]
        ext = 1
        for step, cnt in dims[1:]:
            ext += (cnt - 1) * step
        return [(name, k) for k in range(foff // st, (foff + ext - 1) // st + 1)]

    @contextlib.contextmanager
    def phase(self):
        st = contextlib.ExitStack()
        self.phase_stack = st
        self.nphase += 1
        try:
            yield
        finally:
            self.barrier()
            st.close()
            self.phase_stack = None

    def sb(self, name, shape, dt):
        return self.phase_stack.enter_context(self.nc.sbuf_tensor(f"p{self.nphase}_{name}", list(shape), dt))

    def ps(self, name, shape=(128, 512), dt=F32):
        return self.phase_stack.enter_context(self.nc.psum_tensor(f"p{self.nphase}_{name}", list(shape), dt))

    def barrier(self):
        d = set(self.last_on.values())
        for q in DMAQ:
            lst = [i for i in self.dma_since_bar if self.ops[i].eng == q]
            d |= set(lst[-NDSEM[q]:])
        bar_dst, bar_src = self.bar_aps
        m = self._add("sp", lambda e: e.dma_start(out=bar_dst, in_=bar_src), [], [], rk=[], wk=[], is_dma=True)
        m.deps |= d
        for s in STREAMS:
            self.pending[s] = {m.idx}
        self.dma_since_bar = [m.idx]

    def _add(self, eng, fn, reads, writes, rk=None, wk=None, is_dma=False):
        r = list(rk) if rk is not None else []
        w = list(wk) if wk is not None else []
        if rk is None:
            for a in reads:
                if a is not None and hasattr(a, "tensor"):
                    r += self._keys(a)
        if wk is None:
            for a in writes:
                if a is not None and hasattr(a, "tensor"):
                    w += self._keys(a)
        op = Op(eng, fn, r, w, is_dma)
        op.idx = len(self.ops)
        if self.pending[eng]:
            op.deps |= self.pending[eng]
            self.pending[eng] = set()
        self.ops.append(op)
        if is_dma:
            self.dma_since_bar.append(op.idx)
        else:
            self.last_on[eng] = op.idx
        return op

    def mm(self, out, lhsT, rhs, start=True, stop=True, **kw):
        return self._add("pe", lambda e: e.matmul(out, lhsT, rhs, start=start, stop=stop),
                         [lhsT, rhs], [out], **kw)

    def act(self, out, in_, func, bias=None, scale=None, **kw):
        def fn(e):
            kws = {}
            if bias is not None:
                kws["bias"] = bias
            if scale is not None:
                kws["scale"] = scale
            return e.activation(out, in_, func, **kws)
        return self._add("act", fn, [in_, bias, scale], [out], **kw)

    def tt(self, eng, out, in0, in1, op, **kw):
        return self._add(eng, lambda e: e.tensor_tensor(out, in0, in1, op), [in0, in1], [out], **kw)

    def ts(self, eng, out, in0, s1, op0, s2=None, op1=None, **kw):
        def fn(e):
            if op1 is None:
                return e.tensor_scalar(out, in0, s1, None, op0)
            return e.tensor_scalar(out, in0, s1, s2, op0, op1)
        return self._add(eng, fn, [in0, s1, s2], [out], **kw)

    def stt(self, out, in0, scalar, in1, op0, op1, **kw):
        return self._add("dve", lambda e: e.scalar_tensor_tensor(out, in0, scalar, in1, op0, op1),
                         [in0, scalar, in1], [out], **kw)

    def copy(self, eng, out, in_, **kw):
        if eng == "act":
            return self._add("act", lambda e: e.copy(out, in_), [in_], [out], **kw)
        return self._add(eng, lambda e: e.tensor_copy(out, in_), [in_], [out], **kw)

    def memset(self, eng, ap, val, **kw):
        return self._add(eng, lambda e: e.memset(ap, val), [], [ap], **kw)

    def recip(self, out, in_, **kw):
        return self._add("dve", lambda e: e.reciprocal(out, in_), [in_], [out], **kw)

    def reduce(self, eng, out, in_, op, axis=AX.X, **kw):
        return self._add(eng, lambda e: e.tensor_reduce(out, in_, axis, op), [in_], [out], **kw)

    def scan(self, out, d0, d1, initial, op0, op1, **kw):
        return self._add("dve", lambda e: e.tensor_tensor_scan(out, d0, d1, initial, op0, op1),
                         [d0, d1, initial], [out], **kw)

    def dma(self, q, out, in_, rk=None, wk=None):
        r = [] if _is_dram(in_) else self._keys(in_)
        w = [] if _is_dram(out) else self._keys(out)
        return self._add(q, lambda e: e.dma_start(out=out, in_=in_), [], [], rk=r, wk=w, is_dma=True)

    def _analyze(self):
        ops = self.ops
        last_w = {}
        readers = {}
        for op in ops:
            deps = op.deps
            for k in op.rk:
                if k in last_w:
                    deps.add(last_w[k])
            for k in op.wk:
                if k in last_w:
                    deps.add(last_w[k])
                for r in readers.get(k, ()):
                    deps.add(r)
            deps.discard(op.idx)
            for k in op.rk:
                readers.setdefault(k, []).append(op.idx)
            for k in op.wk:
                last_w[k] = op.idx
                readers[k] = []
        seqc = {}
        for op in ops:
            op.seq = seqc.get(op.eng, 0)
            seqc[op.eng] = op.seq + 1
        dcount = {q: 0 for q in DMAQ}
        dhist = {q: [] for q in DMAQ}
        for op in ops:
            if op.is_dma:
                q = op.eng
                n = NDSEM[q]
                i = dcount[q]
                op.dslot = i % n
                if i >= n:
                    op.deps.add(dhist[q][i - n])
                dhist[q].append(op.idx)
                dcount[q] = i + 1
        clock = {e: {} for e in STREAMS}
        known_dma = {e: set() for e in STREAMS}
        opclock = [None] * len(ops)
        needed = set()
        for op in ops:
            me = op.eng
            ck = clock[me]
            kd = known_dma[me]
            waits = []
            rks = None
            for d in sorted(op.deps):
                dop = ops[d]
                if dop.is_dma:
                    if d in kd:
                        continue
                    waits.append(d)
                    kd.add(d)
                    for s, v in opclock[d].items():
                        if ck.get(s, -1) < v:
                            ck[s] = v
                else:
                    if dop.eng == me:
                        if me == "pe":
                            continue
                        if rks is None:
                            rks = set(op.rk)
                        if not (rks & set(dop.wk)):
                            continue
                        if ck.get(me, -1) >= dop.seq:
                            continue
                        waits.append(d)
                        ck[me] = dop.seq
                        continue
                    if ck.get(dop.eng, -1) >= dop.seq:
                        continue
                    waits.append(d)
                    for s, v in opclock[d].items():
                        if ck.get(s, -1) < v:
                            ck[s] = v
                    ck[dop.eng] = max(ck.get(dop.eng, -1), dop.seq)
            op.waits = waits
            needed.update(waits)
            opclock[op.idx] = dict(ck)
        return needed

    def emit(self, final_wait_ops=()):
        nc = self.nc
        needed = self._analyze()
        for f in final_wait_ops:
            needed.add(f.idx)
        cnt = {e: 0 for e in COMPUTE}
        dcnt = {q: [0] * NDSEM[q] for q in DMAQ}
        for op in self.ops:
            if op.is_dma:
                dcnt[op.eng][op.dslot] += 16
                op.sig = ("d", op.eng, op.dslot, dcnt[op.eng][op.dslot])
            elif op.idx in needed:
                c = cnt[op.eng]
                op.sig = ("c", op.eng, c // EPOCH, c % EPOCH + 1)
                cnt[op.eng] = c + 1
        with contextlib.ExitStack() as st:
            sems = {}
            for e in COMPUTE:
                for ep in range(cnt[e] // EPOCH + 1):
                    sems[("c", e, ep)] = st.enter_context(nc.semaphore(f"s_{e}_{ep}"))
            for q in DMAQ:
                for s in range(NDSEM[q]):
                    sems[("d", q, s)] = st.enter_context(nc.semaphore(f"d_{q}_{s}"))
            block = st.enter_context(nc.Block())
            per = {e: [op for op in self.ops if op.eng == e] for e in STREAMS}
            ops = self.ops

            def run(e, lst, extra_final=()):
                for op in lst:
                    for d in op.waits:
                        sg = ops[d].sig
                        e.wait_ge(sems[sg[:3]], sg[3])
                    ins = op.fn(e)
                    if op.sig is not None:
                        ins.then_inc(sems[op.sig[:3]], 16 if op.is_dma else 1)
                for f in extra_final:
                    sg = f.sig
                    e.wait_ge(sems[sg[:3]], sg[3])

            @block.tensor
            def _(e):
                run(e, per["pe"])

            @block.scalar
            def _(e):
                run(e, per["act"])

            @block.vector
            def _(e):
                run(e, per["dve"])

            @block.gpsimd
            def _(e):
                run(e, sorted(per["pool"] + per["dq"], key=lambda o: o.idx))

            @block.sync
            def _(e):
                run(e, per["sp"], extra_final=final_wait_ops)
        return cnt


D = 1024
SEQ = 4096
NBC = 2
T = NBC * SEQ
CH = 512
NCH = T // CH
CPB = SEQ // CH
DEPTH = 2
EPS = 1e-6
IN_W = 1700
WIN_N = 1808
OFF_AQ, OFF_AK, OFF_AV, OFF_AF, OFF_BU, OFF_BV, OFF_CQ, OFF_CKV, OFF_KR = 0, 256, 512, 768, 772, 1028, 1284, 1540, 1668
OFF_F3, OFF_KRP = 1700, 1712
D_FF = 2752
D_FFE = 1408
NE = 8
MEM = 256

C_ID = 0
C_BD96 = 128
C_PERM = 224
C_SELQ = 320
C_SELK = 600
C_M3 = 880
C_INV = 883
C_SGN = 884
C_MASKA = 885
C_MASKC = 1013
C_SEL65 = 1141
C_SELE = 1205
CW = 1205 + 8 * 128


def build_consts():
    c = np.zeros((128, CW), np.float32)
    c[:, C_ID:C_ID + 128] = np.eye(128, dtype=np.float32)
    c[0:64, C_BD96:C_BD96 + 64] = 1.0 / 64
    c[64:96, C_BD96 + 64:C_BD96 + 96] = 1.0 / 32
    for m in range(96):
        src = m if m < 64 else (m + 16 if m < 80 else m - 16)
        c[src, C_PERM + m] = 1.0
    for h in range(4):
        q0 = C_SELQ + h * 70
        c[12, q0 + 64:q0 + 67] = 1.0
        c[h, q0 + 67] = 1.0
        c[4 + h, q0 + 68] = 1.0
        c[8 + h, q0 + 69] = 1.0
        k0 = C_SELK + h * 70
        c[h, k0 + 64] = -1.0
        c[4 + h, k0 + 65] = -1.0
        c[8 + h, k0 + 66] = -1.0
        c[12, k0 + 67:k0 + 70] = 1.0
    c[0:4, C_M3 + 0] = 1.0
    c[4:8, C_M3 + 1] = 1.0
    c[8:12, C_M3 + 2] = 1.0
    half = 16
    inv = (np.float32(10000.0) ** (-np.arange(half, dtype=np.float32) / np.float32(half))).astype(np.float32)
    c[64:80, C_INV] = inv
    c[80:96, C_INV] = inv
    c[64:80, C_SGN] = -1.0
    c[80:96, C_SGN] = 1.0
    p = np.arange(128)[:, None]
    t = np.arange(128)[None, :]
    c[:, C_MASKA:C_MASKA + 128] = (p <= t).astype(np.float32)
    c[:, C_MASKC:C_MASKC + 128] = ((p // 64) <= (t // 64)).astype(np.float32)
    c[64, C_SEL65:C_SEL65 + 64] = 1.0
    for e in range(8):
        c[e, C_SELE + e * 128:C_SELE + (e + 1) * 128] = 1.0
    return c


PARAM_SPECS = [
    ("mix_norm", (2, 1024)), ("w_in", (2, 1024, 1700)), ("b_forget", (2, 4)), ("a_q_norm", (2, 64)),
    ("a_k_norm", (2, 64)), ("b_v_norm", (2, 256)), ("b_spatial_w", (2, 4, 128, 128)), ("b_spatial_b", (2, 4, 128)),
    ("c_q_lat_norm", (2, 256)), ("c_w_uq", (2, 256, 768)), ("c_kv_lat_norm", (2, 128)), ("c_w_ukv", (2, 128, 1024)),
    ("c_q_nope_norm", (2, 64)), ("c_q_rope_norm", (2, 32)), ("c_k_nope_norm", (2, 64)), ("c_k_rope_norm", (2, 32)),
    ("out_norm_a", (2, 256)), ("out_norm_b", (2, 256)), ("out_norm_c", (2, 512)), ("w_out", (2, 1024, 1024)),
    ("xattn_norm", (2, 1024)), ("mem_norm", (2, 1024)), ("w_mem_q", (2, 1024, 512)), ("w_mem_kv", (2, 1024, 1024)),
    ("m_q_norm", (2, 128)), ("m_k_norm", (2, 128)), ("w_mem_out", (2, 512, 1024)), ("ffn_norm", (2, 1024)),
    ("ffn_w_gate", (1, 1024, 2752)), ("ffn_w_up", (1, 1024, 2752)), ("ffn_w_down", (1, 2752, 1024)),
    ("w_router", (1, 1024, 8)), ("b_router", (1, 8)), ("moe_w_gate", (1, 8, 1024, 1408)),
    ("moe_w_up", (1, 8, 1024, 1408)), ("moe_w_down", (1, 8, 1408, 1024)),
]


def col1(ap1d):
    return ap1d.rearrange("(p o) -> p o", o=1)


class Builder:
    def __init__(self, dbg=None, layers=(0, 1), stop_after=None):
        self.dbg = dbg or ()
        self.stop_after = stop_after
        self.layers = layers
        nc = bass.Bass("TRN2", target_bir_lowering=False)
        self.nc = nc
        self.P = Prog(nc)
        self.I = {}
        self.I["x"] = nc.dram_tensor("x", [T, D], F32, kind="ExternalInput").ap()
        self.I["mem"] = nc.dram_tensor("mem", [NBC * MEM, D], F32, kind="ExternalInput").ap()
        self.I["positions"] = nc.dram_tensor("positions", [NBC, SEQ], I32, kind="ExternalInput").ap()
        self.I["cst"] = nc.dram_tensor("cst", [128, CW], F32, kind="ExternalInput").ap()
        for name, shp in PARAM_SPECS:
            self.I[name] = nc.dram_tensor(name, list(shp), F32, kind="ExternalInput").ap()
        self.out = nc.dram_tensor("out", [T, D], F32, kind="ExternalOutput").ap()
        bar = nc.dram_tensor("bar_scratch", [1, 16], F32, kind="Internal").ap()
        self.P.bar_aps = (bar, self.I["cst"][0:1, 0:16])
        self.S = {}
        self.final_ops = []

    def scratch(self, name, shape, dt):
        kind = "ExternalOutput" if name in self.dbg else "Internal"
        ap = self.nc.dram_tensor(name, list(shape), dt, kind=kind).ap()
        self.S[name] = ap
        return ap

    def build(self):
        P = self.P
        S = self.scratch
        S("xT", [D, T], F32)
        S("hT3", [D, T], BF16)
        S("oT", [D, T], BF16)
        S("qA", [NBC, 70, 4, SEQ], BF16)
        S("kA", [NBC, 70, 4, SEQ], BF16)
        S("vA", [NBC, 128, 4, 32 * 65], BF16)
        S("qC", [NBC, 96, 8, SEQ], BF16)
        S("kC", [NBC, 96, 8, SEQ], BF16)
        S("vC", [NBC, 128, 8, 32 * 65], BF16)
        S("combT", [NE, T], F32)
        for l in range(DEPTH):
            S(f"Win{l}", [8, 128, WIN_N], BF16)
            S(f"Wuq{l}", [2, 128, 768], BF16)
            S(f"Wukv{l}", [1, 128, 1024], BF16)
            S(f"Wout{l}", [8, 128, 1024], BF16)
            S(f"Wmq{l}", [8, 128, 512], BF16)
            S(f"Wmkv{l}", [8, 128, 1024], BF16)
            S(f"Wmo{l}", [4, 128, 1024], BF16)

        stages = []
        stages.append(("P0", self.phase_cast))
        for l in self.layers:
            stages.append((f"P1_{l}", lambda l=l: self.phase_proj(l)))
            stages.append((f"P2_{l}", lambda l=l: self.phase_attn(l)))
            stages.append((f"P3_{l}", lambda l=l: self.phase_mix_out(l)))
            stages.append((f"P4_{l}", lambda l=l: self.phase_ffn_all(l)))
        stages.append(("PF", self.phase_final))
        for name, fn in stages:
            with P.phase():
                fn()
            if self.stop_after == name:
                break
        if not self.final_ops:
            pass
        cnt = P.emit(final_wait_ops=self.final_ops)
        return cnt

    def phase_cast(self):
        P, I, S = self.P, self.I, self.S
        NMAX = WIN_N
        si = [P.sb(f"si{i}", [128, NMAX], F32) for i in range(3)]
        so = [P.sb(f"so{i}", [128, NMAX], BF16) for i in range(3)]
        gcol = P.sb("gcol", [128, 128], F32)
        self._cast_i = 0
        self._gc = 0

        def load_gain(g1d, n):
            cols = []
            for kc in range((n + 127) // 128):
                rows = min(128, n - kc * 128)
                j = self._gc
                self._gc += 1
                P.dma("sp", gcol[0:rows, j:j + 1], col1(g1d[kc * 128:kc * 128 + rows]))
                cols.append(gcol[0:rows, j:j + 1])
            return cols

        def cast(src2d, dst, K, N, gains=None, special=None, Nd=None):
            Nd = Nd or N
            for kc in range((K + 127) // 128):
                rows = min(128, K - kc * 128)
                i = self._cast_i
                self._cast_i += 1
                a, o = si[i % 3], so[i % 3]
                eng = ("dve", "act")[i % 2]
                P.dma("sp", a[0:rows, 0:N], src2d[kc * 128:kc * 128 + rows, :])
                g = gains[kc] if gains is not None else None
                if special is not None:
                    special(a, o, g, rows, eng)
                elif g is None:
                    P.copy(eng, o[0:rows, 0:N], a[0:rows, 0:N])
                elif eng == "act":
                    P.act(o[0:rows, 0:N], a[0:rows, 0:N], AF.Copy, scale=g)
                else:
                    P.ts(eng, o[0:rows, 0:N], a[0:rows, 0:N], g, ALU.mult)
                P.dma("dq", dst[kc, 0:rows, :], o[0:rows, 0:Nd])

        def scaled(eng, out, in_, g):
            if eng == "act":
                P.act(out, in_, AF.Copy, scale=g)
            else:
                P.ts(eng, out, in_, g, ALU.mult)

        for l in range(DEPTH):
            if l not in self.layers:
                continue
            g = load_gain(I["mix_norm"][l], 1024)

            def sp_win(a, o, g, rows, eng):
                scaled(eng, o[:, 0:IN_W], a[:, 0:IN_W], g)
                for r in range(3):
                    P.ts("dve", o[:, OFF_F3 + 4 * r:OFF_F3 + 4 * r + 4], a[:, OFF_AF:OFF_AF + 4], g, ALU.mult)
                P.memset("pool", o[:, OFF_KRP:OFF_KRP + 64], 0.0)
                P.ts("dve", o[:, OFF_KRP + 64:OFF_KRP + 96], a[:, OFF_KR:OFF_KR + 32], g, ALU.mult)
            cast(I["w_in"][l], S[f"Win{l}"], 1024, IN_W, g, special=sp_win, Nd=WIN_N)
            g = load_gain(I["c_q_lat_norm"][l], 256)
            cast(I["c_w_uq"][l], S[f"Wuq{l}"], 256, 768, g)
            g = load_gain(I["c_kv_lat_norm"][l], 128)

            def sp_ukv(a, o, g, rows, eng):
                av = a[:, 0:1024].rearrange("p (h t d) -> p h t d", h=8, t=2)
                for t in range(2):
                    ov = o[:, t * 512:(t + 1) * 512].rearrange("p (h d) -> p h d", h=8)
                    P.ts("dve", ov, av[:, :, t, :], g, ALU.mult)
            cast(I["c_w_ukv"][l], S[f"Wukv{l}"], 128, 1024, g, special=sp_ukv)
            g = load_gain(I["out_norm_a"][l], 256) + load_gain(I["out_norm_b"][l], 256) + load_gain(I["out_norm_c"][l], 512)
            cast(I["w_out"][l], S[f"Wout{l}"], 1024, 1024, g)
            g = load_gain(I["xattn_norm"][l], 1024)
            cast(I["w_mem_q"][l], S[f"Wmq{l}"], 1024, 512, g)
            g = load_gain(I["mem_norm"][l], 1024)
            cast(I["w_mem_kv"][l], S[f"Wmkv{l}"], 1024, 1024, g)
            cast(I["w_mem_out"][l], S[f"Wmo{l}"], 512, 1024, None)

    def load_consts(self):
        P = self.P
        cf = P.sb("cstf", [128, CW], F32)
        cb = P.sb("cstb", [128, CW], BF16)
        P.dma("sp", cf[:], self.I["cst"])
        P.copy("dve", cb[:], cf[:])
        return cf, cb

    def const_tile(self, name, shape, val, dt=BF16):
        t = self.P.sb(name, shape, dt)
        self.P.memset("pool", t[:], val)
        return t

    def rstd_from_ms(self, ms_psum, M, out_rstd, tmp):
        P = self.P
        P.act(tmp, ms_psum, AF.Ln, bias=self.eps_col[0:M, 0:1])
        P.act(out_rstd, tmp, AF.Exp, scale=-0.5)

    def phase_proj(self, l):
        P, I, S = self.P, self.I, self.S
        cf, cb = self.load_consts()
        ident_f = cf[:, C_ID:C_ID + 128]
        self.eps_col = self.const_tile("epsc", [128, 1], EPS, F32)
        halfpi = self.const_tile("halfpi", [128, 1], float(np.pi / 2), F32)
        ones1024 = self.const_tile("on1024", [128, 128], 1.0 / 1024)
        ones256 = self.const_tile("on256", [128, 128], 1.0 / 256)
        ones128 = self.const_tile("on128", [128, 128], 1.0 / 128)
        ones64 = self.const_tile("on64", [64, 64], 1.0 / 64)
        bd96 = cb[0:96, C_BD96:C_BD96 + 96]
        perm96 = cb[0:96, C_PERM:C_PERM + 96]
        Win = P.sb("Win", [128, 8, WIN_N], BF16)
        P.dma("sp", Win[:], S[f"Win{l}"].rearrange("k p n -> p k n"))
        Wuq = P.sb("Wuq", [128, 2, 768], BF16)
        P.dma("sp", Wuq[:], S[f"Wuq{l}"].rearrange("k p n -> p k n"))
        Wukv = P.sb("Wukv", [128, 1024], BF16)
        P.dma("sp", Wukv[:], S[f"Wukv{l}"][0])
        prm = P.sb("prm", [128, 16], F32)
        P.memset("pool", prm[:], 0.0)
        P.dma("sp", prm[0:64, 0:1], col1(I["a_q_norm"][l]))
        P.dma("sp", prm[0:64, 1:2], col1(I["a_k_norm"][l]))
        P.dma("sp", prm[0:64, 2:3], col1(I["c_q_nope_norm"][l]))
        P.dma("sp", prm[64:96, 2:3], col1(I["c_q_rope_norm"][l]))
        P.dma("sp", prm[0:64, 3:4], col1(I["c_k_nope_norm"][l]))
        P.dma("sp", prm[64:96, 4:5], col1(I["c_k_rope_norm"][l]))
        for r in range(3):
            P.dma("sp", prm[4 * r:4 * r + 4, 5:6], col1(I["b_forget"][l]))
        gq_a = P.sb("gq_a", [64, 1], F32)
        P.ts("dve", gq_a[:], prm[0:64, 0:1], 64.0 ** -0.5, ALU.mult)
        gk_a = prm[0:64, 1:2]
        g96q = P.sb("g96q", [96, 1], F32)
        P.ts("dve", g96q[:], prm[0:96, 2:3], 96.0 ** -0.5, ALU.mult)
        gkn = prm[0:64, 3:4]
        g96kr = prm[0:96, 4:5]
        nbf = P.sb("nbf", [12, 1], F32)
        P.ts("dve", nbf[:], prm[0:12, 5:6], -1.0, ALU.mult)
        bvg = P.sb("bvg", [128, 256], F32)
        P.dma("sp", bvg[:], I["b_v_norm"][l:l + 1, :].partition_broadcast(128))
        bsb = P.sb("bsb", [64, 4, 128], F32)
        for g_ in range(4):
            P.dma("sp", bsb[:, g_, :], I["b_spatial_b"][l, g_:g_ + 1, :].partition_broadcast(64))
        wmT = P.sb("wmT", [128, 4, 128], BF16)
        wsf = P.sb("wsf", [128, 4, 128], F32)
        P.dma("sp", wsf[:], I["b_spatial_w"][l].rearrange("g t s -> t g s"))
        NSLOT = 5
        pa = [P.ps(f"pa{i}") for i in range(NSLOT)]
        pm = pa
        px = P.ps("px")
        pv = P.ps("pv", (128, 1024))
        pst = pa[0]
        for g_ in range(4):
            P.mm(pst[:, g_ * 128:(g_ + 1) * 128], wsf[:, g_, :], ident_f)
        maskc = cf[:, C_MASKC:C_MASKC + 128]
        for g_ in range(4):
            P.tt("dve", wmT[:, g_, :], pst[:, g_ * 128:(g_ + 1) * 128], maskc, ALU.mult)
        xtm = [P.sb(f"xtm{i}", [128, D], F32) for i in range(2)] if l == 0 else None
        xc = [P.sb(f"xc{i}", [128, 8, CH], F32) for i in range(1 if l == 0 else 2)]
        sqx = [P.sb(f"sqx{i}", [128, CH], BF16) for i in range(2)]
        hT = P.split1(P.sb("hT", [128, 8, CH], BF16), CH)
        qf = [P.sb(f"qf{i}", [128, CH], F32) for i in range(NSLOT)]
        sqt = [P.sb(f"sqt{i}", [128, CH], BF16) for i in range(NSLOT)]
        rs_t = [P.sb(f"rs_t{i}", [128, CH], F32) for i in range(NSLOT)]
        qn_t = [P.sb(f"qn_t{i}", [96, CH], BF16) for i in range(NSLOT)]
        rstd_x = rs_t[0]

        qaugA = P.split1(P.sb("qaugA", [70, 4, CH], BF16), CH)
        kaugA = P.split1(P.sb("kaugA", [70, 4, CH], BF16), CH)
        vAt = P.sb("vAt", [128, 4, 4, 65], BF16)
        vCt = P.sb("vCt", [128, 8, 4, 65], BF16)
        P.memset("pool", vAt[:], 1.0)
        P.memset("pool", vCt[:], 1.0)
        qCt = P.split1(P.sb("qCt", [96, 8, CH], BF16), CH)
        kCt = P.split1(P.sb("kCt", [96, 8, CH], BF16), CH)
        oBt = P.split1(P.sb("oBt", [64, 4, CH], BF16), CH)
        cqn = P.sb("cqn", [128, 2, CH], BF16)
        ckvn = P.sb("ckvn", [128, CH], BF16)
        krf = P.sb("krf", [96, CH], BF16)
        Fs = [P.sb(f"Fs{i}", [12, CH], F32) for i in range(2)]
        ef = P.sb("ef", [12, CH], F32)
        onesf = self.const_tile("onesf", [12, CH], 1.0, F32)
        hi = P.sb("hi", [12, CH], BF16)
        lo = P.sb("lo", [12, CH], BF16)
        lo2 = P.sb("lo2", [12, CH], BF16)
        r1 = P.sb("r1", [12, CH], F32)
        c1 = P.sb("c1", [12, CH], F32)
        Faug = self.const_tile("Faug", [13, CH], 1.0, BF16)
        m3 = cf[0:12, C_M3:C_M3 + 3]
        posi = P.sb("posi", [96, CH], I32)
        ang = P.sb("ang", [96, CH], F32)
        ang2 = P.sb("ang2", [96, CH], F32)
        uu = P.sb("uu", [96, CH], F32)
        Ctab = P.sb("Ctab", [96, CH], F32)
        Stab = P.sb("Stab", [96, CH], F32)
        ug = P.split1(P.sb("ug", [64, 4, CH], BF16), CH)
        vg = P.sb("vg", [128, 16, 64], F32)
        xm = P.sb("xm", [128, 16, 64], F32)
        s1 = P.sb("s1", [128, 16], F32)
        s2 = P.sb("s2", [128, 16], F32)
        vnb = P.sb("vnb", [128, 16, 64], BF16)
        yb = P.sb("yb", [64, CH], F32)

        def proj(out_ps, col0, ncols, rhs3=None):
            rhs3 = rhs3 if rhs3 is not None else hT
            for k in range(8):
                P.mm(out_ps, Win[:, k, col0:col0 + ncols], rhs3[:, k, :], start=(k == 0), stop=(k == 7))

        def run_chains(chains):
            queue = list(chains)
            active = []
            free = list(range(NSLOT))
            while queue or active:
                while queue and len(free) >= queue[0][0]:
                    n, fac = queue.pop(0)
                    sl = [free.pop() for _ in range(n)]
                    active.append((fac(sl), sl))
                for item in list(active):
                    try:
                        next(item[0])
                    except StopIteration:
                        active.remove(item)
                        free.extend(item[1])

        def chain_norm(slots, src_fn, M, ones_lhsT, gain_col, out_bf, rope_out=None, after=None):
            sl = slots[0]
            ps_ = pa[sl]
            src_fn(ps_)
            yield
            q = qf[sl]
            P.copy("act", q[0:M, :], ps_[0:M, :])
            P.tt("dve", sqt[sl][0:M, :], q[0:M, :], q[0:M, :], ALU.mult)
            yield
            ms = pm[sl]
            P.mm(ms[0:M, :], ones_lhsT, sqt[sl][0:M, :])
            yield
            rs = rs_t[sl]
            self.rstd_from_ms(ms[0:M, :], M, rs[0:M, :], rs[0:M, :])
            dst = out_bf if rope_out is None else qn_t[sl][:]
            if gain_col is None:
                P.tt("dve", dst, q[0:M, :], rs[0:M, :], ALU.mult)
            else:
                P.stt(dst, q[0:M, :], gain_col, rs[0:M, :], ALU.mult, ALU.mult)
            if rope_out is not None:
                yield
                qn = qn_t[sl][:]
                pq = pm[sl]
                P.mm(pq[0:96, :], perm96, qn)
                t1 = rs_t[sl][0:96, :]
                t2 = qf[sl][0:96, :]
                P.tt("pool", t1, qn, Ctab[:], ALU.mult)
                yield
                P.tt("dve", t2, pq[0:96, :], Stab[:], ALU.mult)
                P.tt("pool" if sl % 2 == 1 else "dve", rope_out, t1, t2, ALU.add)
            if after is not None:
                after()

        for cg in range(NCH):
            b, c = divmod(cg, CPB)
            c0 = cg * CH
            cb0 = c * CH
            xcc = xc[cg % len(xc)]
            if l == 0:
                for tb in range(4):
                    xt_ = xtm[tb % 2]
                    P.dma("sp", xt_[:], I["x"][c0 + tb * 128:c0 + (tb + 1) * 128, :])
                    for k in range(0, 8, 4):
                        ps_ = pa[(2 * tb + k // 4) % NSLOT]
                        for kk in range(4):
                            P.mm(ps_[:, kk * 128:(kk + 1) * 128], xt_[:, (k + kk) * 128:(k + kk + 1) * 128], ident_f)
                        dst = xcc[:, k:k + 4, tb * 128:(tb + 1) * 128]
                        P.copy("act" if (k // 4 + tb) % 2 == 0 else "dve", dst,
                               ps_[:].rearrange("p (k t) -> p k t", k=4))
                P.dma("dq", S["xT"].rearrange("(k p) t -> p k t", p=128)[:, :, c0:c0 + CH], xcc[:],
                      wk=[("xT", cg)])
            else:
                P.dma("sp", xcc[:], S["xT"].rearrange("(k p) t -> p k t", p=128)[:, :, c0:c0 + CH],
                      rk=[("xT", cg)])
            ms = px
            for k in range(8):
                sq_ = sqx[k % 2]
                P.tt("dve" if k % 2 == 0 else "pool", sq_[:], xcc[:, k, :], xcc[:, k, :], ALU.mult)
                P.mm(ms[:], ones1024[:], sq_[:], start=(k == 0), stop=(k == 7))
            self.rstd_from_ms(ms[:], 128, rstd_x[:], rstd_x[:])
            for k in range(8):
                P.tt("dve" if k % 2 == 0 else "pool", hT[:, k, :], xcc[:, k, :], rstd_x[:], ALU.mult)
            P.dma("sp", posi[:], I["positions"][b:b + 1, cb0:cb0 + CH].partition_broadcast(96))
            P.copy("dve", ang[:], posi[:])
            P.ts("dve", ang[:], ang[:], cf[0:96, C_INV:C_INV + 1], ALU.mult)
            P.ts("dve", ang2[:], ang[:], float(np.pi / 2), ALU.add)
            for (a_, tab, sgn) in ((ang, Stab, True), (ang2, Ctab, False)):
                P.ts("dve", uu[:], a_[:], float(1.0 / (2 * np.pi)), ALU.mult)
                P.copy("dve", posi[:], uu[:])
                P.copy("dve", uu[:], posi[:])
                P.stt(a_[:], uu[:], -6.28125, a_[:], ALU.mult, ALU.add)
                P.stt(a_[:], uu[:], float(-(2 * np.pi - 6.28125)), a_[:], ALU.mult, ALU.add)
                P.ts("dve", a_[:], a_[:], 3.1415925, ALU.min, -3.1415925, ALU.max)
                P.act(tab[:], a_[:], AF.Sin)
                if sgn:
                    P.ts("dve", tab[:], tab[:], cf[0:96, C_SGN:C_SGN + 1], ALU.mult)
            def ch_forget(slots):
                sl = slots[0]
                pf_ = pa[sl]
                proj(pf_[0:12, :], OFF_F3, 12)
                yield
                P.act(ef[:], pf_[0:12, :], AF.Exp, bias=nbf[:], scale=-1.0)
                P.act(ef[:], ef[:], AF.Ln, bias=1.0)
                Fcur = Fs[cg % 2]
                init = 0.0 if c == 0 else Fs[(cg - 1) % 2][:, CH - 1:CH]
                P.scan(Fcur[:], onesf[:], ef[:], init, ALU.mult, ALU.subtract)
                yield
                P.copy("dve", hi[:], Fcur[:])
                P.tt("dve", r1[:], Fcur[:], hi[:], ALU.subtract)
                P.copy("dve", lo[:], r1[:])
                P.tt("dve", r1[:], r1[:], lo[:], ALU.subtract)
                P.copy("dve", lo2[:], r1[:])
                P.ts("dve", c1[:], hi[:], m3[:, 0:1], ALU.mult)
                P.stt(c1[:], lo[:], m3[:, 1:2], c1[:], ALU.mult, ALU.add)
                P.stt(Faug[0:12, :], lo2[:], m3[:, 2:3], c1[:], ALU.mult, ALU.add)
                yield
                i = 0
                for h in range(4):
                    for (dst, selc) in ((qaugA, C_SELQ), (kaugA, C_SELK)):
                        pss = pm[sl] if i % 2 == 0 else pa[sl]
                        i += 1
                        P.mm(pss[0:70, :], cb[0:13, selc + h * 70:selc + (h + 1) * 70], Faug[:])
                        yield
                        P.copy("act", dst[64:70, h, :], pss[64:70, :])

            def ch_av(slots):
                for tb in range(4):
                    for k in range(8):
                        P.mm(pv[:, tb * 256:(tb + 1) * 256], hT[:, k, tb * 128:(tb + 1) * 128],
                             Win[:, k, OFF_AV:OFF_AV + 256], start=(k == 0), stop=(k == 7))
                    if tb % 2 == 1:
                        yield
                for tb in range(4):
                    P.copy("act" if tb % 2 == 0 else "dve", vAt[:, :, tb, 0:64],
                           pv[:, tb * 256:(tb + 1) * 256].rearrange("p (h d) -> p h d", h=4))
                yield

            def ch_bu(slots, g_):
                sl = slots[0]
                ps_ = pa[sl]
                proj(ps_[0:64, :], OFF_BU + g_ * 64, 64)
                yield
                P.act(ug[:, g_, :], ps_[0:64, :], AF.Gelu)

            def ch_bv(slots):
                for tb in range(4):
                    for k in range(8):
                        P.mm(pv[:, tb * 256:(tb + 1) * 256], hT[:, k, tb * 128:(tb + 1) * 128],
                             Win[:, k, OFF_BV:OFF_BV + 256], start=(k == 0), stop=(k == 7))
                    if tb % 2 == 1:
                        yield
                P.act(vg[:].rearrange("p a d -> p (a d)"), pv[:], AF.Gelu)
                P.reduce("dve", s1[:], vg[:], ALU.add)
                P.ts("dve", s1[:], s1[:], 1.0 / 64, ALU.mult)
                yield
                P.tt("dve", xm[:], vg[:], s1[:].unsqueeze(2).to_broadcast([128, 16, 64]), ALU.subtract)
                P.tt("pool", vg[:], xm[:], xm[:], ALU.mult)
                yield
                P.reduce("dve", s2[:], vg[:], ALU.add)
                P.act(s2[:], s2[:], AF.Ln, bias=self.eps_col[:, 0:1], scale=1.0 / 64)
                P.act(s2[:], s2[:], AF.Exp, scale=-0.5)
                yield
                P.tt("dve", xm[:], xm[:], s2[:].unsqueeze(2).to_broadcast([128, 16, 64]), ALU.mult)
                P.tt("pool", vnb[:].rearrange("p (a g) d -> p a (g d)", a=4), xm[:].rearrange("p (a g) d -> p a (g d)", a=4),
                     bvg[:].unsqueeze(1).to_broadcast([128, 4, 256]), ALU.mult)
                yield
                sl = slots[0]
                for g_ in range(4):
                    py = pm[sl] if g_ % 2 == 0 else pa[sl]
                    for tb in range(4):
                        P.mm(py[0:64, tb * 128:(tb + 1) * 128], vnb[:, tb * 4 + g_, :], wmT[:, g_, :])
                    yield
                    P.tt("dve", yb[:].rearrange("p (a t) -> p a t", a=4), py[0:64, :].rearrange("p (a t) -> p a t", a=4),
                         bsb[:, g_, :].unsqueeze(1).to_broadcast([64, 4, 128]), ALU.add)
                    P.tt("pool", oBt[:, g_, :], yb[:], ug[:, g_, :], ALU.mult)

            def ch_cq(slots):
                s0, s1_ = slots
                proj(pa[s0][:], OFF_CQ, 128)
                proj(pa[s1_][:], OFF_CQ + 128, 128)
                yield
                P.copy("act", qf[s0][:], pa[s0][:])
                P.copy("act", qf[s1_][:], pa[s1_][:])
                P.tt("pool", sqt[s0][:], qf[s0][:], qf[s0][:], ALU.mult)
                P.tt("pool", sqt[s1_][:], qf[s1_][:], qf[s1_][:], ALU.mult)
                yield
                ms = pm[s0]
                P.mm(ms[:], ones256[:], sqt[s0][:], start=True, stop=False)
                P.mm(ms[:], ones256[:], sqt[s1_][:], start=False, stop=True)
                yield
                self.rstd_from_ms(ms[:], 128, rs_t[s0][:], rs_t[s0][:])
                P.tt("dve", cqn[:, 0, :], qf[s0][:], rs_t[s0][:], ALU.mult)
                P.tt("dve", cqn[:, 1, :], qf[s1_][:], rs_t[s0][:], ALU.mult)

            def src_q(h):
                def f(ps_):
                    for m in range(2):
                        P.mm(ps_[0:96, :], Wuq[:, m, h * 96:(h + 1) * 96], cqn[:, m, :], start=(m == 0), stop=(m == 1))
                return f

            def src_k(h):
                return lambda ps_: P.mm(ps_[0:64, :], Wukv[:, h * 64:(h + 1) * 64], ckvn[:])

            def ch_cv(slots):
                for tb in range(4):
                    half = pv[:, (tb % 2) * 512:(tb % 2 + 1) * 512]
                    P.mm(half, ckvn[:, tb * 128:(tb + 1) * 128], Wukv[:, 512:1024])
                    yield
                    P.copy("act" if tb % 2 == 0 else "dve", vCt[:, :, tb, 0:64],
                           half.rearrange("p (h d) -> p h d", h=8))

            chains = [(1, ch_forget), (2, ch_cq),
                      (1, lambda sl: chain_norm(sl, lambda ps_: proj(ps_[:], OFF_CKV, 128), 128, ones128[:], None, ckvn[:])),
                      (1, lambda sl: chain_norm(sl, lambda ps_: proj(ps_[0:96, :], OFF_KRP, 96), 96, bd96, g96kr, None,
                                                rope_out=krf[:]))]
            for h in range(4):
                chains.append((1, lambda sl, h=h: chain_norm(sl, lambda ps_: proj(ps_[0:64, :], OFF_AQ + h * 64, 64), 64,
                                                             ones64[:], gq_a[:], qaugA[0:64, h, :])))
                chains.append((1, lambda sl, h=h: chain_norm(sl, lambda ps_: proj(ps_[0:64, :], OFF_AK + h * 64, 64), 64,
                                                             ones64[:], gk_a, kaugA[0:64, h, :])))
            chains.append((0, ch_av))
            for g_ in range(4):
                chains.append((1, lambda sl, g_=g_: ch_bu(sl, g_)))
            chains.append((1, ch_bv))
            for h in range(8):
                chains.append((1, lambda sl, h=h: chain_norm(sl, src_q(h), 96, bd96, g96q[:], None, rope_out=qCt[:, h, :])))
                chains.append((1, lambda sl, h=h: chain_norm(
                    sl, src_k(h), 64, ones64[:], gkn, kCt[0:64, h, :],
                    after=(lambda: P.copy("act" if h % 2 == 0 else "pool", kCt[64:96, h, :], krf[64:96, :])))))
                if h == 3:
                    chains.append((0, ch_cv))
            run_chains(chains)
            P.dma("dq", S["qA"][b, :, :, cb0:cb0 + CH], qaugA[:])
            P.dma("dq", S["kA"][b, :, :, cb0:cb0 + CH], kaugA[:])
            P.dma("dq", S["qC"][b, :, :, cb0:cb0 + CH], qCt[:])
            P.dma("dq", S["kC"][b, :, :, cb0:cb0 + CH], kCt[:])
            P.dma("dq", S["vA"][b].rearrange("p h (n d) -> p h n d", d=65)[:, :, 4 * c:4 * c + 4, :], vAt[:])
            P.dma("dq", S["vC"][b].rearrange("p h (n d) -> p h n d", d=65)[:, :, 4 * c:4 * c + 4, :], vCt[:])
            P.dma("dq", S["oT"][256:512, :].rearrange("(g p) t -> p g t", p=64)[:, :, c0:c0 + CH], oBt[:],
                  wk=[("oT", cg)])

    def phase_attn(self, l):
        P, I, S = self.P, self.I, self.S
        cf, cb = self.load_consts()
        maskA = cb[:, C_MASKA:C_MASKA + 128]
        maskC = cb[:, C_MASKC:C_MASKC + 128]
        sel65 = cf[0:65, C_SEL65:C_SEL65 + 64]
        qt = [P.sb(f"qt{i}", [96, SEQ], BF16) for i in range(2)]
        kt = [P.sb(f"kt{i}", [96, SEQ], BF16) for i in range(2)]
        vt = [P.sb(f"vt{i}", [128, 32, 65], BF16) for i in range(2)]
        NST = 5
        pst_ = [P.ps(f"st{i}") for i in range(NST)]
        po = [P.ps("po0"), P.ps("po1")]
        prs = P.ps("prs")
        PT = [P.sb(f"PT{i}", [128, CH], BF16) for i in range(NST)]
        of = [P.sb(f"of{i}", [65, CH], F32) for i in range(2)]
        rinv = [P.sb(f"rinv{i}", [64, CH], F32) for i in range(2)]
        ob = [P.sb(f"ob{i}", [64, CH], BF16) for i in range(2)]
        hidx = 0
        for b in range(NBC):
            for (kind, nh, dk, qs, ks, vs, mask, row0) in (("A", 4, 70, "qA", "kA", "vA", maskA, 0),
                                                           ("C", 8, 96, "qC", "kC", "vC", maskC, 512)):
                for h in range(nh):
                    q_, k_, v_ = qt[hidx % 2], kt[hidx % 2], vt[hidx % 2]
                    hidx += 1
                    P.dma("sp", q_[0:dk, :], S[qs][b, :, h, :])
                    P.dma("sp", k_[0:dk, :], S[ks][b, :, h, :])
                    P.dma("sp", v_[:].rearrange("p n d -> p (n d)"), S[vs][b, :, h, :])
                    pairs = []
                    for c in range(CPB):
                        for j in range(4 * c + 4):
                            pairs.append((c, j))
                    LOOK = 3
                    npair = len(pairs)

                    def score(i):
                        c, j = pairs[i]
                        r = j - 4 * c
                        q0 = c * CH + (128 * r if r > 0 else 0)
                        n = CH - (128 * r if r > 0 else 0)
                        st_ = pst_[i % NST]
                        P.mm(st_[:, 0:n], k_[0:dk, j * 128:(j + 1) * 128], q_[0:dk, q0:q0 + n])
                        pt_ = PT[i % NST]
                        P.act(pt_[:, 0:n], st_[:, 0:n], AF.Exp)
                        if r >= 0:
                            P.tt("dve", pt_[:, 0:128], pt_[:, 0:128], mask, ALU.mult)

                    def pv_(i):
                        c, j = pairs[i]
                        r = j - 4 * c
                        off = (128 * r if r > 0 else 0)
                        n = CH - off
                        o_ = po[c % 2]
                        last = (j == 4 * c + 3)
                        P.mm(o_[0:65, off:CH], v_[:, j, :], PT[i % NST][:, 0:n], start=(j == 0), stop=last)
                        if last:
                            of_ = of[c % 2]
                            P.copy("dve", of_[:], o_[0:65, :])

                            def fin(c=c, of_=of_):
                                P.mm(prs[0:64, :], sel65, of_[:])
                                ri = rinv[c % 2]
                                P.recip(ri[:], prs[0:64, :])
                                ob_ = ob[c % 2]
                                P.tt("dve", ob_[:], of_[0:64, :], ri[:], ALU.mult)
                                cg = b * CPB + c
                                r0 = row0 + h * 64
                                P.dma("dq", S["oT"][r0:r0 + 64, cg * CH:(cg + 1) * CH], ob_[:])
                            deferred.append([3, fin])

                    deferred = []
                    for i in range(npair + LOOK):
                        if i < npair:
                            score(i)
                        if i >= LOOK:
                            pv_(i - LOOK)
                        for d_ in list(deferred):
                            d_[0] -= 1
                            if d_[0] <= 0:
                                d_[1]()
                                deferred.remove(d_)
                    for d_ in deferred:
                        d_[1]()

    def phase_mix_out(self, l):
        P, I, S = self.P, self.I, self.S
        cf, cb = self.load_consts()
        ident_f = cf[:, C_ID:C_ID + 128]
        self.eps_col = self.const_tile("epsc", [128, 1], EPS, F32)
        ones1024 = self.const_tile("on1024", [128, 128], 1.0 / 1024)
        ones512 = self.const_tile("on512", [128, 128], 1.0 / 512)
        ones256 = self.const_tile("on256", [128, 128], 1.0 / 256)
        ones128 = self.const_tile("on128", [128, 128], 1.0 / 128)
        ones1 = self.const_tile("on1", [128, 128], 1.0)
        Wout = P.sb("Wout", [128, 8, 1024], BF16)
        P.dma("sp", Wout[:], S[f"Wout{l}"].rearrange("k p n -> p k n"))
        Wmq = P.sb("Wmq", [128, 8, 512], BF16)
        P.dma("sp", Wmq[:], S[f"Wmq{l}"].rearrange("k p n -> p k n"))
        Wmo = P.sb("Wmo", [128, 4, 1024], BF16)
        P.dma("sp", Wmo[:], S[f"Wmo{l}"].rearrange("k p n -> p k n"))
        Wmkv = P.sb("Wmkv", [128, 8, 1024], BF16)
        P.dma("sp", Wmkv[:], S[f"Wmkv{l}"].rearrange("k p n -> p k n"))
        prm = P.sb("prm", [128, 4], F32)
        P.dma("sp", prm[:, 0:1], col1(I["m_q_norm"][l]))
        P.dma("sp", prm[:, 1:2], col1(I["m_k_norm"][l]))
        gmq = P.sb("gmq", [128, 1], F32)
        P.ts("dve", gmq[:], prm[:, 0:1], 128.0 ** -0.5, ALU.mult)
        gmk = prm[:, 1:2]
        moe = (l % 2 == 1)
        if moe:
            Wr = P.sb("Wr", [128, 8, 8], F32)
            P.dma("sp", Wr[:], I["w_router"][0].rearrange("(k p) e -> p k e", p=128))
            gf = P.sb("gf", [128, 8], F32)
            for k in range(8):
                P.dma("sp", gf[:, k:k + 1], col1(I["ffn_norm"][l, k * 128:(k + 1) * 128]))
            for k in range(8):
                P.ts("dve", Wr[:, k, :], Wr[:, k, :], gf[:, k:k + 1], ALU.mult)
            brt = P.sb("brt", [128, 8], F32)
            P.dma("sp", brt[:], I["b_router"][0:1, :].partition_broadcast(128))
        bk = [P.ps(f"bk{i}") for i in range(8)]
        self._i = {}

        def nxt(lst, key):
            i = self._i.get(key, 0)
            self._i[key] = i + 1
            return lst[i % len(lst)]

        def nb():
            return nxt(bk, "bk")

        xc = [P.sb(f"xc{i}", [128, 8, CH], F32) for i in range(1 if moe else 2)]
        oc = [P.sb(f"oc{i}", [128, 8, CH], BF16) for i in range(2)]
        sqx = [P.sb(f"sqx{i}", [128, CH], BF16) for i in range(4)]
        on = P.split1(P.sb("on", [128, 8, CH], BF16), CH)
        h2T = P.split1(P.sb("h2T", [128, 8, CH], BF16), CH)
        h3T = P.sb("h3T", [128, 8, CH], BF16)
        h3f = P.sb("h3f", [128, 8, CH], F32) if moe else None
        rstd = [P.sb(f"rstd{i}", [128, CH], F32) for i in range(3)]
        qfh = [P.sb(f"qfh{i}", [128, CH], F32) for i in range(4)]
        sqh = [P.sb(f"sqh{i}", [128, CH], BF16) for i in range(4)]
        rsh = [P.sb(f"rsh{i}", [128, CH], F32) for i in range(4)]
        qnh = [P.sb(f"qnh{i}", [128, CH], BF16) for i in range(4)]
        PTh = [[P.sb(f"PT{i}_{j}", [128, CH], BF16) for j in range(2)] for i in range(4)]
        om = P.split1(P.sb("om", [128, 4, CH], BF16), CH)
        KmT = P.sb("KmT", [128, 4, MEM], BF16)
        Vm = P.sb("Vm", [128, 2, 512], BF16)
        memt = P.sb("memt", [128, D], F32)
        memT = P.sb("memT", [128, 8, MEM], F32)
        msq = P.sb("msq", [128, 8, MEM], BF16)
        mnT = P.sb("mnT", [128, 8, MEM], BF16)
        if moe:
            lg = P.sb("lg", [128, 4, 8], F32)
            eq1 = P.sb("eq1", [128, 4, 8], F32)
            eq2 = P.sb("eq2", [128, 4, 8], F32)
            l2 = P.sb("l2", [128, 4, 8], F32)
            sc = P.sb("sc", [128, 8, 4], F32)
            comb = P.sb("comb", [128, 4, 8], F32)
            cTt = P.sb("cTt", [8, CH], F32)

        def ms_accum(src3, nk, ones_lhsT, bank, n=CH):
            for k in range(nk):
                sq_ = nxt(sqx, "sqx")
                P.tt("dve" if k % 2 == 0 else "pool", sq_[:, 0:n], src3[:, k, :], src3[:, k, :], ALU.mult)
                P.mm(bank[:, 0:n], ones_lhsT, sq_[:, 0:n], start=(k == 0), stop=(k == nk - 1))

        def rmsT(src3, ones_lhsT, out3, nk, out_f32=None):
            n = src3.shape[2]
            ms = nb()
            ms_accum(src3, nk, ones_lhsT, ms, n)
            rs = nxt(rstd, "rstd")
            self.rstd_from_ms(ms[:, 0:n], 128, rs[:, 0:n], rs[:, 0:n])
            for k in range(nk):
                P.tt("dve" if k % 2 == 0 else "pool", out3[:, k, :], src3[:, k, :], rs[:, 0:n], ALU.mult)
                if out_f32 is not None:
                    P.tt("pool" if k % 2 == 0 else "dve", out_f32[:, k, :], src3[:, k, :], rs[:, 0:n], ALU.mult)

        def run_rr(gens):
            active = list(gens)
            while active:
                for g in list(active):
                    try:
                        next(g)
                    except StopIteration:
                        active.remove(g)

        for cg in range(NCH):
            b, c = divmod(cg, CPB)
            c0 = cg * CH
            if c == 0:
                for mb in range(2):
                    P.dma("sp", memt[:], I["mem"][b * MEM + mb * 128:b * MEM + (mb + 1) * 128, :])
                    for k in range(0, 8, 4):
                        ps_ = nb()
                        for kk in range(4):
                            P.mm(ps_[:, kk * 128:(kk + 1) * 128], memt[:, (k + kk) * 128:(k + kk + 1) * 128], ident_f)
                        P.copy("act", memT[:, k:k + 4, mb * 128:(mb + 1) * 128], ps_[:].rearrange("p (k t) -> p k t", k=4))
                P.tt("pool", msq[:], memT[:], memT[:], ALU.mult)
                ms = nb()
                for k in range(8):
                    P.mm(ms[:, 0:MEM], ones1024[:], msq[:, k, :], start=(k == 0), stop=(k == 7))
                rs = nxt(rstd, "rstd")
                self.rstd_from_ms(ms[:, 0:MEM], 128, rs[:, 0:MEM], rs[:, 0:MEM])
                for k in range(8):
                    P.tt("dve", mnT[:, k, :], memT[:, k, :], rs[:, 0:MEM], ALU.mult)
                for h in range(4):
                    ps_ = nb()
                    for k in range(8):
                        P.mm(ps_[:, 0:MEM], Wmkv[:, k, h * 128:(h + 1) * 128], mnT[:, k, :], start=(k == 0), stop=(k == 7))
                    qf_ = qfh[h]
                    P.copy("act", qf_[:, 0:MEM], ps_[:, 0:MEM])
                    sq_ = sqh[h]
                    P.tt("pool", sq_[:, 0:MEM], qf_[:, 0:MEM], qf_[:, 0:MEM], ALU.mult)
                    ms = nb()
                    P.mm(ms[:, 0:MEM], ones128[:], sq_[:, 0:MEM])
                    rs = rsh[h]
                    self.rstd_from_ms(ms[:, 0:MEM], 128, rs[:, 0:MEM], rs[:, 0:MEM])
                    P.stt(KmT[:, h, :], qf_[:, 0:MEM], gmk, rs[:, 0:MEM], ALU.mult, ALU.mult)
                for mb in range(2):
                    ps_ = nb()
                    for k in range(8):
                        P.mm(ps_[:], mnT[:, k, mb * 128:(mb + 1) * 128], Wmkv[:, k, 512:1024], start=(k == 0), stop=(k == 7))
                    P.copy("act", Vm[:, mb, :], ps_[:])
            xcc = xc[cg % len(xc)]
            occ = oc[cg % 2]
            P.dma("sp", occ[:], S["oT"].rearrange("(k p) t -> p k t", p=128)[:, :, c0:c0 + CH])
            P.dma("sp", xcc[:], S["xT"].rearrange("(k p) t -> p k t", p=128)[:, :, c0:c0 + CH])
            groups = ((0, 2, ones256), (2, 2, ones256), (4, 4, ones512))
            gms = []
            for (k0, nk, ones_) in groups:
                ms = nb()
                ms_accum(occ[:, k0:k0 + nk, :], nk, ones_[:], ms)
                gms.append(ms)
            grs = []
            for gi in range(3):
                rs = nxt(rstd, "rstd")
                self.rstd_from_ms(gms[gi][:], 128, rs[:], rs[:])
                grs.append(rs)
            for gi, (k0, nk, ones_) in enumerate(groups):
                for k in range(nk):
                    P.tt("dve" if k % 2 == 0 else "pool", on[:, k0 + k, :], occ[:, k0 + k, :], grs[gi][:], ALU.mult)
            for m in range(8):
                ps_ = nb()
                for k in range(8):
                    P.mm(ps_[:], Wout[:, k, m * 128:(m + 1) * 128], on[:, k, :], start=(k == 0), stop=(k == 7))
                P.tt("dve", xcc[:, m, :], xcc[:, m, :], ps_[:], ALU.add)
            rmsT(xcc[:], ones1024[:], h2T, 8)

            def head(h):
                b0, b1 = bk[2 * h], bk[2 * h + 1]
                for k in range(8):
                    P.mm(b0[:], Wmq[:, k, h * 128:(h + 1) * 128], h2T[:, k, :], start=(k == 0), stop=(k == 7))
                yield
                qf_ = qfh[h]
                P.copy("act", qf_[:], b0[:])
                P.tt("pool" if h % 2 == 0 else "dve", sqh[h][:], qf_[:], qf_[:], ALU.mult)
                yield
                P.mm(b1[:], ones128[:], sqh[h][:])
                yield
                rs = rsh[h]
                self.rstd_from_ms(b1[:], 128, rs[:], rs[:])
                qn = qnh[h]
                P.stt(qn[:], qf_[:], gmq[:], rs[:], ALU.mult, ALU.mult)
                yield
                P.mm(b0[:], KmT[:, h, 0:128], qn[:])
                P.mm(b1[:], KmT[:, h, 128:256], qn[:])
                yield
                P.act(PTh[h][0][:], b0[:], AF.Exp)
                P.act(PTh[h][1][:], b1[:], AF.Exp)
                yield
                for mb in range(2):
                    P.mm(b0[:], Vm[:, mb, h * 128:(h + 1) * 128], PTh[h][mb][:], start=(mb == 0), stop=(mb == 1))
                for mb in range(2):
                    P.mm(b1[:], ones1[:], PTh[h][mb][:], start=(mb == 0), stop=(mb == 1))
                yield
                ri = qfh[h]
                P.recip(ri[:], b1[:])
                P.tt("dve", om[:, h, :], b0[:], ri[:], ALU.mult)

            run_rr([head(h) for h in range(4)])
            for m in range(8):
                ps_ = nb()
                for h in range(4):
                    P.mm(ps_[:], Wmo[:, h, m * 128:(m + 1) * 128], om[:, h, :], start=(h == 0), stop=(h == 3))
                P.tt("dve", xcc[:, m, :], xcc[:, m, :], ps_[:], ALU.add)
            P.dma("dq", S["xT"].rearrange("(k p) t -> p k t", p=128)[:, :, c0:c0 + CH], xcc[:])
            rmsT(xcc[:], ones1024[:], h3T, 8, out_f32=h3f)
            P.dma("dq", S["hT3"].rearrange("(k p) t -> p k t", p=128)[:, :, c0:c0 + CH], h3T[:])
            if moe:
                pl = nb()
                for tb in range(4):
                    for k in range(8):
                        P.mm(pl[:, tb * 8:(tb + 1) * 8], h3f[:, k, tb * 128:(tb + 1) * 128], Wr[:, k, :],
                             start=(k == 0), stop=(k == 7))
                B3 = [128, 4, 8]
                P.tt("dve", lg[:], pl[:, 0:32].rearrange("p (a e) -> p a e", a=4), brt[:].unsqueeze(1).to_broadcast(B3), ALU.add)
                m1, m2, dd, ee, den, g1, g2 = (sc[:, i, :] for i in range(7))
                P.reduce("dve", m1, lg[:], ALU.max)
                P.tt("dve", eq1[:], lg[:], m1.unsqueeze(2).to_broadcast(B3), ALU.is_equal)
                P.stt(l2[:], eq1[:], -1e30, lg[:], ALU.mult, ALU.add)
                P.reduce("dve", m2, l2[:], ALU.max)
                P.tt("dve", eq2[:], l2[:], m2.unsqueeze(2).to_broadcast(B3), ALU.is_equal)
                P.tt("dve", dd, m2, m1, ALU.subtract)
                P.act(ee, dd, AF.Exp)
                P.ts("dve", den, ee, 1.0, ALU.add)
                P.recip(g1, den)
                P.tt("dve", g2, ee, g1, ALU.mult)
                P.tt("dve", comb[:], eq1[:], g1.unsqueeze(2).to_broadcast(B3), ALU.mult)
                P.tt("dve", eq2[:], eq2[:], g2.unsqueeze(2).to_broadcast(B3), ALU.mult)
                P.tt("dve", comb[:], comb[:], eq2[:], ALU.add)
                pc = nb()
                for tb in range(4):
                    P.mm(pc[0:8, tb * 128:(tb + 1) * 128], comb[:, tb, :], ident_f)
                P.copy("act", cTt[:], pc[0:8, :])
                P.dma("dq", S["combT"][:, c0:c0 + CH], cTt[:])

    def phase_ffn_all(self, l):
        P, I, S = self.P, self.I, self.S
        moe = (l % 2 == 1)
        if not moe:
            passes = [(I["ffn_w_gate"][l // 2], I["ffn_w_up"][l // 2], I["ffn_w_down"][l // 2], 0, 1408, None),
                      (I["ffn_w_gate"][l // 2], I["ffn_w_up"][l // 2], I["ffn_w_down"][l // 2], 1408, 1344, None)]
        else:
            passes = [(I["moe_w_gate"][l // 2, e], I["moe_w_up"][l // 2, e], I["moe_w_down"][l // 2, e], 0, 1408, e)
                      for e in range(NE)]
        NH, NJ = 1408, 11
        Wg = [P.sb(f"Wg{i}", [128, 8, NH], BF16) for i in range(2)]
        Wu = [P.sb(f"Wu{i}", [128, 8, NH], BF16) for i in range(2)]
        Wd = [P.sb(f"Wd{i}", [128, NJ, D], BF16) for i in range(2)]
        stg = [P.sb(f"stg{i}", [128, NH], F32) for i in range(2)]
        gcol = P.sb("gcol", [128, 8], F32)
        for k in range(8):
            P.dma("sp", gcol[:, k:k + 1], col1(I["ffn_norm"][l, k * 128:(k + 1) * 128]))
        if moe:
            selt = P.sb("selt", [8, NE * 128], F32)
            P.dma("sp", selt[:], I["cst"][0:8, C_SELE:C_SELE + NE * 128])
            cTt = [P.sb(f"cTt{i}", [8, CH], F32) for i in range(2)]
            cbt = [P.sb(f"cbt{i}", [128, CH], F32) for i in range(2)]
            pcb = P.ps("pcb")
        pg = [P.ps("pg0"), P.ps("pg1")]
        pu = [P.ps("pu0"), P.ps("pu1")]
        py = [P.ps("py0"), P.ps("py1")]
        xc = [P.sb(f"xc{i}", [128, 8, CH], F32) for i in range(1)]
        hc = [P.sb(f"hc{i}", [128, 8, CH], BF16) for i in range(2)]
        at = [P.split1(P.sb(f"at{i}", [128, NJ, CH], BF16), CH) for i in range(1)]
        sg = [P.sb(f"sg{i}", [128, CH], F32) for i in range(2)]
        xTv = S["xT"].rearrange("(k p) t -> p k t", p=128)
        hTv = S["hT3"].rearrange("(k p) t -> p k t", p=128)
        self._stg_i = 0

        def prep_tasks(pi):
            wg_, wu_, wd_, hid0, nhid, _ = passes[pi]
            bi = pi % 2
            for (src, dst) in ((wg_, Wg[bi]), (wu_, Wu[bi])):
                for k in range(8):
                    st_ = stg[self._stg_i % 2]
                    self._stg_i += 1
                    P.dma("sp", st_[:, 0:nhid], src[k * 128:(k + 1) * 128, hid0:hid0 + nhid])
                    if self._stg_i % 2 == 0:
                        P.ts("dve", dst[:, k, 0:nhid], st_[:, 0:nhid], gcol[:, k:k + 1], ALU.mult)
                    else:
                        P.act(dst[:, k, 0:nhid], st_[:, 0:nhid], AF.Copy, scale=gcol[:, k:k + 1])
                    yield
            nj = (nhid + 127) // 128
            for j in range(nj):
                rows = min(128, nhid - j * 128)
                st_ = stg[self._stg_i % 2]
                self._stg_i += 1
                P.dma("sp", st_[0:rows, 0:D], wd_[hid0 + j * 128:hid0 + j * 128 + rows, :])
                P.copy("act" if self._stg_i % 2 == 0 else "dve", Wd[bi][0:rows, j, :], st_[0:rows, 0:D])
                yield

        for _ in prep_tasks(0):
            pass
        n = 0
        for pi, (wg_, wu_, wd_, hid0, nhid, expert) in enumerate(passes):
            bi = pi % 2
            nj = (nhid + 127) // 128
            nxt_prep = prep_tasks(pi + 1) if pi + 1 < len(passes) else iter(())
            for cg in range(NCH):
                c0 = cg * CH
                xcc, hcc, a_ = xc[0], hc[cg % 2], at[0]
                if expert is not None:
                    ct = cTt[cg % 2]
                    P.dma("sp", ct[:], S["combT"][:, c0:c0 + CH])
                P.dma("sp", hcc[:], hTv[:, :, c0:c0 + CH])
                P.dma("sp", xcc[:], xTv[:, :, c0:c0 + CH])
                if expert is not None:
                    P.mm(pcb[:], selt[:, expert * 128:(expert + 1) * 128], ct[:])
                    cbc = cbt[cg % 2]
                    P.copy("act", cbc[:], pcb[:])
                for j in range(nj):
                    rows = min(128, nhid - j * 128)
                    g_, u_ = pg[j % 2], pu[j % 2]
                    for k in range(8):
                        P.mm(g_[0:rows, :], Wg[bi][:, k, j * 128:j * 128 + rows], hcc[:, k, :], start=(k == 0), stop=(k == 7))
                    for k in range(8):
                        P.mm(u_[0:rows, :], Wu[bi][:, k, j * 128:j * 128 + rows], hcc[:, k, :], start=(k == 0), stop=(k == 7))
                    s_ = sg[n % 2]
                    n += 1
                    P.act(s_[0:rows, :], g_[0:rows, :], AF.Silu)
                    if expert is None:
                        P.tt("dve", a_[0:rows, j, :], s_[0:rows, :], u_[0:rows, :], ALU.mult)
                    else:
                        P.tt("dve", s_[0:rows, :], s_[0:rows, :], u_[0:rows, :], ALU.mult)
                        P.tt("pool", a_[0:rows, j, :], s_[0:rows, :], cbc[0:rows, :], ALU.mult)
                for m in range(8):
                    y_ = py[m % 2]
                    for j in range(nj):
                        rows = min(128, nhid - j * 128)
                        P.mm(y_[:], Wd[bi][0:rows, j, m * 128:(m + 1) * 128], a_[0:rows, j, :],
                             start=(j == 0), stop=(j == nj - 1))
                    P.tt("dve", xcc[:, m, :], xcc[:, m, :], y_[:], ALU.add)
                P.dma("dq", xTv[:, :, c0:c0 + CH], xcc[:])
                for _ in range(2):
                    next(nxt_prep, None)
            for _ in nxt_prep:
                pass

    def phase_final(self):
        P, I, S = self.P, self.I, self.S
        cf = P.sb("cstf", [128, CW], F32)
        P.dma("sp", cf[:], I["cst"])
        ident_f = cf[:, C_ID:C_ID + 128]
        xc = [P.sb(f"xc{i}", [128, 8, CH], F32) for i in range(2)]
        ot = [P.sb(f"ot{i}", [128, D], F32) for i in range(3)]
        pp = [P.ps(f"pp{i}", (128, 1024)) for i in range(3)]
        xTv = S["xT"].rearrange("(k p) t -> p k t", p=128)
        n = 0
        for cg in range(NCH):
            c0 = cg * CH
            xcc = xc[cg % 2]
            P.dma("sp", xcc[:], xTv[:, :, c0:c0 + CH], rk=[("xT", cg)])
            for tb in range(4):
                p_ = pp[n % 3]
                o_ = ot[n % 3]
                n += 1
                for k in range(8):
                    P.mm(p_[:, k * 128:(k + 1) * 128], xcc[:, k, tb * 128:(tb + 1) * 128], ident_f)
                P.copy("act", o_[:, 0:512], p_[:, 0:512])
                P.copy("dve", o_[:, 512:1024], p_[:, 512:1024])
                self.final_ops.append(P.dma("dq", self.out[c0 + tb * 128:c0 + (tb + 1) * 128, :], o_[:]))


_CACHE = {}


def _get_builder():
    if "b" not in _CACHE:
        b = Builder()
        b.build()
        _CACHE["b"] = b
    return _CACHE["b"]


def make_in_maps(inputs, n_cores=8):
    cst = build_consts()
    x = np.ascontiguousarray(inputs["x"], dtype=np.float32)
    mem = np.ascontiguousarray(inputs["mem"], dtype=np.float32)
    pos = np.ascontiguousarray(inputs["positions"], dtype=np.int32)
    shared = {"cst": cst}
    for name, shp in PARAM_SPECS:
        shared[name] = np.ascontiguousarray(inputs[name], dtype=np.float32).reshape(shp)
    maps = []
    for i in range(n_cores):
        m = dict(shared)
        m["x"] = x[NBC * i:NBC * (i + 1)].reshape(T, D)
        m["mem"] = mem[NBC * i:NBC * (i + 1)].reshape(NBC * MEM, D)
        m["positions"] = pos[NBC * i:NBC * (i + 1)]
        maps.append(m)
    return maps


def kernel(**inputs):
    b = _get_builder()
    maps = make_in_maps(inputs)
    res = run_bass_kernel_spmd(b.nc, maps, core_ids=list(range(8)))
    outs = [np.asarray(r["out"]).reshape(NBC, SEQ, D) for r in res.results]
    return np.concatenate(outs, axis=0).astype(np.float32)
```
